# Optimizing a Trainium2 kernel written in Bass

```python
import math
import jax, jax.numpy as jnp
from jax import lax
import numpy as np


D_MODEL = 1024
BATCH = 8
SEQ = 4096
DEPTH = 1

D_MIX = 2 * D_MODEL
GMLP_WIDTH = D_MIX // 2
GMLP_HEAD_DIM = 128
GMLP_HEADS = GMLP_WIDTH // GMLP_HEAD_DIM
GMLP_CHUNK = 128
SSD_WIDTH = D_MIX - GMLP_WIDTH
SSD_HEAD_DIM = 64
SSD_HEADS = SSD_WIDTH // SSD_HEAD_DIM
SSD_GROUPS = 2
SSD_STATE = 128
SSD_CHUNK = 128
CONV_WIDTH = 5
CONV_CH = SSD_WIDTH + 2 * SSD_GROUPS * SSD_STATE
DT_MIN = 1e-3
DT_MAX = 1e-1
IN_COLS = 2 * GMLP_WIDTH + SSD_WIDTH + CONV_CH + 2 * SSD_HEADS
IN_SPLITS = [2 * GMLP_WIDTH, 2 * GMLP_WIDTH + SSD_WIDTH, 2 * GMLP_WIDTH + SSD_WIDTH + CONV_CH]
N_EGROUPS = 4
EXPERTS_PER_GROUP = 8
N_EXPERTS = N_EGROUPS * EXPERTS_PER_GROUP
TOP_K_IN_GROUP = 2
D_EXPERT = D_MODEL // 4
N_MOD = 6
EPS = 1e-6

kernel_name = 'hymba_gmlp_ssd_hiermoe_adaln_encoder'


def rmsnorm(x, g):
    xf = x.astype(jnp.float32)
    y = xf * lax.rsqrt(jnp.mean(xf * xf, axis=-1, keepdims=True) + EPS)
    return y.astype(x.dtype) * g


def layernorm(x, g, b):
    xf = x.astype(jnp.float32)
    mu = jnp.mean(xf, axis=-1, keepdims=True)
    var = jnp.mean(jnp.square(xf - mu), axis=-1, keepdims=True)
    y = (xf - mu) * lax.rsqrt(var + EPS)
    return y.astype(x.dtype) * g + b


def gmlp_mixer(zg, ln_g, ln_b, w_s, b_s, out_g):
    bn, s, _ = zg.shape
    zg = jax.nn.gelu(zg, approximate=False)
    u, v = jnp.split(zg, 2, axis=-1)
    v = layernorm(v, ln_g, ln_b)
    nc = s // GMLP_CHUNK
    v = v.reshape(bn, nc, GMLP_CHUNK, GMLP_HEADS, GMLP_HEAD_DIM)
    mixed = jnp.einsum('hij,bnjhd->bnihd', w_s, v) + b_s.T[:, :, None]
    out = u * mixed.reshape(bn, s, GMLP_WIDTH)
    return rmsnorm(out, out_g)


def ssd_chunked(x, dt, a, bm, cm):
    b, s, h, p = x.shape
    g, n = bm.shape[2], bm.shape[3]
    r = h // g
    nc = s // SSD_CHUNK
    xr = (x * dt[..., None]).reshape(b, nc, SSD_CHUNK, g, r, p)
    da = (dt * a).reshape(b, nc, SSD_CHUNK, g, r)
    bc = bm.reshape(b, nc, SSD_CHUNK, g, n)
    cc = cm.reshape(b, nc, SSD_CHUNK, g, n)
    a_cs = jnp.cumsum(jnp.moveaxis(da, 2, -1), axis=-1)
    lower = jnp.tril(jnp.ones((SSD_CHUNK, SSD_CHUNK), dtype=bool))
    seg = a_cs[..., :, None] - a_cs[..., None, :]
    decay_in = jnp.exp(jnp.where(lower, seg, -jnp.inf))
    scores = jnp.einsum('bclgn,bcsgn->bcgls', cc, bc)
    y_diag = jnp.einsum('bcgls,bcgrls,bcsgrp->bclgrp', scores, decay_in, xr)
    decay_to_end = jnp.exp(a_cs[..., -1:] - a_cs)
    states = jnp.einsum('bclgn,bcgrl,bclgrp->bcgrpn', bc, decay_to_end, xr)
    chunk_decay = jnp.exp(a_cs[..., -1])

    def step(carry, inp):
        st, dec = inp
        return carry * dec[..., None, None] + st, carry

    init = jnp.zeros_like(states[:, 0])
    _, prev = lax.scan(step, init, (jnp.moveaxis(states, 1, 0), jnp.moveaxis(chunk_decay, 1, 0)))
    prev = jnp.moveaxis(prev, 0, 1)
    y_off = jnp.einsum('bclgn,bcgrpn,bcgrl->bclgrp', cc, prev, jnp.exp(a_cs))
    return (y_diag + y_off).reshape(b, s, h, p)


def ssd_mixer(z, xbc, dt_raw, conv_w, conv_b, a_log_f, a_log_b, dt_bias_f, dt_bias_b, d_skip, norm_g):
    bn, s, _ = xbc.shape
    f32 = jnp.float32
    xbc = lax.conv_general_dilated(xbc, conv_w[:, None, :], window_strides=(1,),
                                   padding=[(CONV_WIDTH // 2, CONV_WIDTH // 2)],
                                   dimension_numbers=('NWC', 'WIO', 'NWC'),
                                   feature_group_count=CONV_CH)
    xbc = jax.nn.silu(xbc + conv_b).astype(f32)
    xs, bm, cm = jnp.split(xbc, [SSD_WIDTH, SSD_WIDTH + SSD_GROUPS * SSD_STATE], axis=-1)
    xs = xs.reshape(bn, s, SSD_HEADS, SSD_HEAD_DIM)
    bm = bm.reshape(bn, s, SSD_GROUPS, SSD_STATE)
    cm = cm.reshape(bn, s, SSD_GROUPS, SSD_STATE)
    dt_f, dt_b = jnp.split(dt_raw.astype(f32), 2, axis=-1)
    dt_f = jax.nn.softplus(dt_f + dt_bias_f.astype(f32))
    dt_b = jax.nn.softplus(dt_b + dt_bias_b.astype(f32))
    a_f = -jnp.exp(a_log_f.astype(f32))
    a_b = -jnp.exp(a_log_b.astype(f32))
    y_f = ssd_chunked(xs, dt_f, a_f, bm, cm)
    y_b = jnp.flip(ssd_chunked(jnp.flip(xs, 1), jnp.flip(dt_b, 1), a_b,
                               jnp.flip(bm, 1), jnp.flip(cm, 1)), 1)
    y = y_f + y_b + d_skip.astype(f32)[:, None] * xs
    y = y.reshape(bn, s, SSD_WIDTH) * jax.nn.silu(z.astype(f32))
    y = y.reshape(bn, s, SSD_GROUPS, SSD_WIDTH // SSD_GROUPS)
    y = y * lax.rsqrt(jnp.mean(y * y, axis=-1, keepdims=True) + EPS)
    return y.reshape(bn, s, SSD_WIDTH).astype(z.dtype) * norm_g


def hier_moe(h, w_rg, b_rg, w_re, b_re, w1, w3, w2):
    bn, s, d = h.shape
    t = h.reshape(-1, d)
    f32 = jnp.float32
    g_logits = (t @ w_rg).astype(f32) + b_rg.astype(f32)
    g_prob = jax.nn.softmax(g_logits, axis=-1)
    g_idx = jnp.argmax(g_logits, axis=-1)
    p_g = jnp.max(g_prob, axis=-1, keepdims=True)
    e_logits = jnp.einsum('td,gde->tge', t, w_re).astype(f32) + b_re.astype(f32)
    e_sel = jnp.einsum('tge,tg->te', e_logits, jax.nn.one_hot(g_idx, N_EGROUPS, dtype=f32))
    top_v, top_i = lax.top_k(e_sel, TOP_K_IN_GROUP)
    w_k = jax.nn.softmax(top_v, axis=-1) * p_g
    eid = g_idx[:, None] * EXPERTS_PER_GROUP + top_i
    comb = jnp.sum(jax.nn.one_hot(eid, N_EXPERTS, dtype=f32) * w_k[..., None], axis=1)
    comb = comb.astype(t.dtype)
    out = jnp.zeros_like(t)
    for e in range(N_EXPERTS):
        a = jax.nn.silu(t @ w1[e]) * (t @ w3[e])
        out = out + comb[:, e:e + 1] * (a @ w2[e])
    return out.reshape(bn, s, d)


def setup_inputs(seed: int = 0) -> dict:
    key = jax.random.key(seed)
    ks = jax.random.split(key, 32)
    nrm = jax.random.normal
    L = DEPTH

    def gain(k, shape):
        return 1.0 + 0.01 * nrm(k, shape, jnp.float32)

    def small(k, shape):
        return 0.01 * nrm(k, shape, jnp.float32)

    dt = jnp.exp(jax.random.uniform(ks[14], (L, SSD_HEADS)) * (math.log(DT_MAX) - math.log(DT_MIN)) + math.log(DT_MIN))
    dt_bias_f = dt + jnp.log(-jnp.expm1(-dt))
    dt2 = jnp.exp(jax.random.uniform(ks[15], (L, SSD_HEADS)) * (math.log(DT_MAX) - math.log(DT_MIN)) + math.log(DT_MIN))
    dt_bias_b = dt2 + jnp.log(-jnp.expm1(-dt2))
    return {
        'x': nrm(ks[0], (BATCH, SEQ, D_MODEL), jnp.float32),
        'c': nrm(ks[1], (BATCH, D_MODEL), jnp.float32),
        'w_ada': nrm(ks[2], (L, D_MODEL, N_MOD * D_MODEL), jnp.float32) * D_MODEL ** -0.5,
        'b_ada': small(ks[3], (L, N_MOD * D_MODEL)),
        'norm1_g': gain(ks[4], (L, D_MODEL)),
        'w_in': nrm(ks[5], (L, D_MODEL, IN_COLS), jnp.float32) * D_MODEL ** -0.5,
        'b_in': small(ks[6], (L, IN_COLS)),
        'gmlp_ln_g': gain(ks[7], (L, GMLP_WIDTH)),
        'gmlp_ln_b': small(ks[8], (L, GMLP_WIDTH)),
        'gmlp_w_s': nrm(ks[9], (L, GMLP_HEADS, GMLP_CHUNK, GMLP_CHUNK), jnp.float32) * GMLP_CHUNK ** -0.5,
        'gmlp_b_s': gain(ks[10], (L, GMLP_HEADS, GMLP_CHUNK)),
        'gmlp_out_g': gain(ks[11], (L, GMLP_WIDTH)),
        'conv_w': nrm(ks[12], (L, CONV_WIDTH, CONV_CH), jnp.float32) * CONV_WIDTH ** -0.5,
        'conv_b': small(ks[13], (L, CONV_CH)),
        'a_log_f': jnp.log(jax.random.uniform(ks[16], (L, SSD_HEADS), jnp.float32, 1.0, 16.0)),
        'a_log_b': jnp.log(jax.random.uniform(ks[17], (L, SSD_HEADS), jnp.float32, 1.0, 16.0)),
        'dt_bias_f': dt_bias_f,
        'dt_bias_b': dt_bias_b,
        'd_skip': gain(ks[18], (L, SSD_HEADS)),
        'ssd_norm_g': gain(ks[19], (L, SSD_WIDTH)),
        'w_out': nrm(ks[20], (L, D_MIX, D_MODEL), jnp.float32) * D_MIX ** -0.5,
        'norm2_g': gain(ks[21], (L, D_MODEL)),
        'w_router_g': nrm(ks[22], (L, D_MODEL, N_EGROUPS), jnp.float32) * D_MODEL ** -0.5,
        'b_router_g': small(ks[23], (L, N_EGROUPS)),
        'w_router_e': nrm(ks[24], (L, N_EGROUPS, D_MODEL, EXPERTS_PER_GROUP), jnp.float32) * D_MODEL ** -0.5,
        'b_router_e': small(ks[25], (L, N_EGROUPS, EXPERTS_PER_GROUP)),
        'w1': nrm(ks[26], (L, N_EXPERTS, D_MODEL, D_EXPERT), jnp.float32) * D_MODEL ** -0.5,
        'w3': nrm(ks[27], (L, N_EXPERTS, D_MODEL, D_EXPERT), jnp.float32) * D_MODEL ** -0.5,
        'w2': nrm(ks[28], (L, N_EXPERTS, D_EXPERT, D_MODEL), jnp.float32) * D_EXPERT ** -0.5,
        'final_g': gain(ks[29], (D_MODEL,)),
    }


def reference(x, c, w_ada, b_ada, norm1_g, w_in, b_in, gmlp_ln_g, gmlp_ln_b, gmlp_w_s, gmlp_b_s,
              gmlp_out_g, conv_w, conv_b, a_log_f, a_log_b, dt_bias_f, dt_bias_b, d_skip, ssd_norm_g,
              w_out, norm2_g, w_router_g, b_router_g, w_router_e, b_router_e, w1, w3, w2, final_g):
    c_act = jax.nn.silu(c)
    for l in range(DEPTH):
        mod = (c_act @ w_ada[l] + b_ada[l])[:, None, :]
        shift1, scale1, gate1, shift2, scale2, gate2 = jnp.split(mod, N_MOD, axis=-1)
        h = rmsnorm(x, norm1_g[l]) * (1.0 + scale1) + shift1
        proj = h @ w_in[l] + b_in[l]
        zg, z, xbc, dt_raw = jnp.split(proj, IN_SPLITS, axis=-1)
        y_a = gmlp_mixer(zg, gmlp_ln_g[l], gmlp_ln_b[l], gmlp_w_s[l], gmlp_b_s[l], gmlp_out_g[l])
        y_b = ssd_mixer(z, xbc, dt_raw, conv_w[l], conv_b[l], a_log_f[l], a_log_b[l],
                        dt_bias_f[l], dt_bias_b[l], d_skip[l], ssd_norm_g[l])
        mix = jnp.concatenate([y_a, y_b], axis=-1)
        x = x + gate1 * (mix @ w_out[l])
        h = rmsnorm(x, norm2_g[l]) * (1.0 + scale2) + shift2
        x = x + gate2 * hier_moe(h, w_router_g[l], b_router_g[l], w_router_e[l], b_router_e[l],
                                 w1[l], w3[l], w2[l])
    return rmsnorm(x, final_g)
```

```python
import contextlib
import threading
import numpy as np
import ml_dtypes
import concourse.bass as bass
import concourse.mybir as mybir
from concourse.bass_utils import run_bass_kernel_spmd

F32 = mybir.dt.float32
BF16 = mybir.dt.bfloat16
ALU = mybir.AluOpType
AF = mybir.ActivationFunctionType
AX = mybir.AxisListType

D = 1024
NCORES = 8
EPS = 1e-6
NEG = -30000.0
ENGS = ("pe", "act", "dve", "pool", "sp")
HND = {"pe": "tensor", "act": "scalar", "dve": "vector", "pool": "gpsimd", "sp": "sync"}


class Op:
    __slots__ = ("eng", "fn", "deps", "sig", "is_dma", "dsem", "dval", "grp")


class Prog:
    def __init__(self, nc, same_engine_sync=True):
        self.nc = nc
        self.ops = {e: [] for e in ENGS}
        self.last_w = {}
        self.readers = {}
        self.ses = same_engine_sync
        self.dma_sems = {}
        self.base = contextlib.ExitStack()
        self.esem = {e: self.base.enter_context(nc.semaphore("e_" + e)) for e in ENGS}
        self.cnt = {e: 0 for e in ENGS}
        self.waited = {e: {} for e in ENGS}
        self.scope = self.base
        self._il = None
        self._tl = threading.local()

    def stream(self):
        return getattr(self._tl, "idx", None) if self._il is not None else None

    def _next_turn(self, i):
        st = self._il["st"]
        n = len(st["done"])
        for d in range(1, n + 1):
            j = (i + d) % n
            if not st["done"][j]:
                st["turn"] = j
                return
        st["turn"] = -1

    def interleave(self, fns, quanta):
        n = len(fns)
        st = {"turn": 0, "done": [False] * n, "err": None}
        cv = threading.Condition()
        self._il = {"st": st, "cv": cv, "quanta": quanta, "used": [0] * n}

        def runner(i):
            self._tl.idx = i
            with cv:
                while st["turn"] != i:
                    cv.wait()
            try:
                fns[i]()
            except BaseException as ex:
                st["err"] = ex
            with cv:
                st["done"][i] = True
                self._next_turn(i)
                cv.notify_all()
        ths = [threading.Thread(target=runner, args=(i,)) for i in range(n)]
        for t in ths:
            t.start()
        for t in ths:
            t.join()
        self._il = None
        if st["err"] is not None:
            raise st["err"]

    def _yield(self):
        il = self._il
        if il is None:
            return
        i = getattr(self._tl, "idx", None)
        if i is None:
            return
        il["used"][i] += 1
        if il["used"][i] > il["quanta"][i]:
            il["used"][i] = 1
            st, cv = il["st"], il["cv"]
            with cv:
                self._next_turn(i)
                cv.notify_all()
                while st["turn"] != i:
                    cv.wait()

    def sbuf(self, name, shape, dtype):
        return self.scope.enter_context(self.nc.sbuf_tensor(name, list(shape), dtype))

    def psum(self, name, shape, dtype):
        return self.scope.enter_context(self.nc.psum_tensor(name, list(shape), dtype))

    @staticmethod
    def _k(k):
        if isinstance(k, tuple):
            return tuple(Prog._k(x) for x in k)
        if isinstance(k, (str, int)):
            return k
        return k.name

    def op(self, eng, fn, reads=(), writes=(), _noyield=False):
        if not _noyield:
            self._yield()
        reads = [self._k(k) for k in reads]
        writes = [self._k(k) for k in writes]
        o = Op()
        o.eng, o.fn, o.is_dma, o.sig, o.dsem, o.dval = eng, fn, False, None, None, 0
        deps = []
        for k in reads:
            w = self.last_w.get(k)
            if w is not None:
                deps.append(w)
        for k in writes:
            w = self.last_w.get(k)
            if w is not None:
                deps.append(w)
            deps.extend(self.readers.get(k, ()))
        o.deps = [(d.dsem, self.dma_sems[d.grp][1]) if d.is_dma else d for d in deps]
        self.ops[eng].append(o)
        for k in reads:
            self.readers.setdefault(k, []).append(o)
        for k in writes:
            self.last_w[k] = o
            self.readers[k] = []
        return o

    def dma(self, eng, fn, group, reads=(), writes=()):
        self._yield()
        o = self.op(eng, fn, reads, writes, _noyield=True)
        o.is_dma = True
        group = self._k(group)
        if group not in self.dma_sems:
            self.dma_sems[group] = [self.base.enter_context(self.nc.semaphore("d%d" % len(self.dma_sems))), 0]
        ent = self.dma_sems[group]
        ent[1] += 16
        o.dsem, o.dval, o.grp = ent[0], ent[1], group
        return o

    def _skip_same(self, d, e):
        return d.eng == e and (not self.ses or e in ("pe", "sp"))

    def flush(self):
        for e in ENGS:
            for o in self.ops[e]:
                for d in o.deps:
                    if isinstance(d, tuple) or self._skip_same(d, o.eng):
                        continue
                    if d.sig is None:
                        d.sig = 0
            for o in reversed(self.ops[e]):
                if not o.is_dma:
                    if o.sig is None:
                        o.sig = 0
                    break
        for e in ENGS:
            for o in self.ops[e]:
                if o.sig == 0:
                    self.cnt[e] += 1
                    o.sig = self.cnt[e]
        with self.nc.Block() as block:
            for e in ENGS:
                def body(h, e=e, ops=self.ops[e]):
                    waited = self.waited[e]
                    for o in ops:
                        need = {}
                        for d in o.deps:
                            if isinstance(d, tuple):
                                sem, val = d
                                key = ("d", sem.name if hasattr(sem, "name") else id(sem))
                            else:
                                if not d.sig or self._skip_same(d, e):
                                    continue
                                key, sem, val = ("e", d.eng), self.esem[d.eng], d.sig
                            if waited.get(key, 0) >= val:
                                continue
                            if key not in need or need[key][1] < val:
                                need[key] = (sem, val)
                        for key, (sem, val) in need.items():
                            h.wait_ge(sem, val)
                            waited[key] = val
                        ins = o.fn(h)
                        if o.is_dma:
                            ins.then_inc(o.dsem, 16)
                        elif o.sig:
                            ins.then_inc(self.esem[e], 1)
                    for e2 in ENGS:
                        if e2 != e and self.cnt[e2] > waited.get(("e", e2), 0):
                            h.wait_ge(self.esem[e2], self.cnt[e2])
                            waited[("e", e2)] = self.cnt[e2]
                    for g, (sem, tot) in self.dma_sems.items():
                        key = ("d", sem.name if hasattr(sem, "name") else id(sem))
                        if tot > waited.get(key, 0):
                            h.wait_ge(sem, tot)
                            waited[key] = tot
                getattr(block, HND[e])(body)
        self.ops = {e: [] for e in ENGS}
        self.last_w = {}
        self.readers = {}


def bcl(ap, shape):
    return ap.unsqueeze(2).to_broadcast(list(shape))


def bcm(ap, shape):
    return ap.unsqueeze(1).to_broadcast(list(shape))


def build_nc(S, debug=None, stop=None):
    TL = 256
    CPT = TL // 128
    NT = S // TL
    NCH = S // 128
    TS = min(S, 1024)
    NSUP = S // TS
    CHS = TS // 128
    nc = bass.Bass("TRN2", target_bir_lowering=False)

    def din(name, shape, dt=F32):
        return nc.dram_tensor(name, list(shape), dt, kind="ExternalInput").ap()

    x = din("x", [S, D])
    ccol = din("ccol", [128, 8])
    w_ada = din("w_ada", [D, 6 * D])
    badac = din("badac", [128, 48])
    n12g = din("n12g", [128, 16])
    w_in = din("w_in", [D, 4640])
    b_in = din("b_in", [1, 4640])
    bxbc = din("bxbc", [128, 12])
    lngb = din("lngb", [2, D])
    w_sT = din("w_sT", [128, 8, 128])
    b_s2 = din("b_s2", [128, 8])
    ngcol = din("ngcol", [128, 16])
    convw = din("convw", [128, 12, 5])
    convb = din("convb", [128, 12])
    ssdv = din("ssdv", [1, 64])
    dskc = din("dskc", [128, 8])
    w_out = din("w_out", [2 * D, D])
    w_r = din("w_r", [D, 36])
    b_r = din("b_r", [1, 36])
    w13p = din("w13p", [32 * 128 * 2, 2048])
    w2p = din("w2p", [32 * 128, 2048])
    c2_d = din("c2", [128, 34])
    fing = din("fing", [1, D])
    cbf_d = din("cbf", [128, 6, 128], BF16)
    cf_d = din("cf", [128, 5, 128])
    out = nc.dram_tensor("out", [S, D], F32, kind="ExternalOutput").ap()
    skind = "ExternalOutput" if debug else "Internal"
    prevb_d = nc.dram_tensor("prevb_s", [NCH, 128, D], BF16, kind=skind).ap()
    x1_d = nc.dram_tensor("x1_s", [S, D], F32, kind=skind).ap()
    yT_d = nc.dram_tensor("yT_s", [NCH, 128, 16, 128], BF16, kind="Internal").ap()
    w13c_d = nc.dram_tensor("w13c_s", [32 * 128 * 2, 2048], BF16, kind="Internal").ap()
    w2c_d = nc.dram_tensor("w2c_s", [32 * 128, 2048], BF16, kind="Internal").ap()

    def dump(name, tap, keys):
        if not debug:
            return
        t = nc.dram_tensor("dbg_" + name, list(tap.shape), tap.dtype, kind="ExternalOutput").ap()
        P.dma("sp", lambda e: e.dma_start(out=t, in_=tap), "dbg_" + name, reads=keys)

    P = Prog(nc)
    op, dma = P.op, P.dma

    cbf = P.sbuf("cbf_t", [128, 6, 128], BF16)
    cf = P.sbuf("cf_t", [128, 5, 128], F32)
    ident_bf, ones_bf, U_bf, L_bf, mF_bf, mB_bf = [cbf[:, i, :] for i in range(6)]
    ident_f, ones_f, U_f, L_f, SL_f = [cf[:, i, :] for i in range(5)]
    modc = P.sbuf("modc", [128, 48], F32)
    G12 = P.sbuf("G12", [128, 16], F32)
    n12 = P.sbuf("n12", [128, 16], F32)
    junkb = P.sbuf("junkb", [128, D], BF16)
    junk = junkb
    PS = P.psum("PS", [128, 8 * 512], F32)
    psn = [0, 0, 0, 0]

    def pbank(n=1):
        sid = P.stream()
        if sid is None:
            b = psn[0]
            if b + n > 8:
                b = 0
            psn[0] = (b + n) % 8
        else:
            lo, nb = ((0, 4), (4, 2), (6, 2))[sid]
            b = psn[1 + sid]
            if b + n > nb:
                b = 0
            psn[1 + sid] = (b + n) % nb
            b += lo
        return b, PS[:, b * 512:(b + n) * 512], [("ps", b + i) for i in range(n)]

    dma("sp", lambda e: e.dma_start(out=cbf[:], in_=cbf_d[:, :, :]), "c0", writes=[cbf])
    dma("sp", lambda e: e.dma_start(out=cf[:], in_=cf_d[:, :, :]), "c1", writes=[cf])
    dma("sp", lambda e: e.dma_start(out=n12[:], in_=n12g[:, :]), "c2", writes=[n12])

    ph12 = contextlib.ExitStack()
    P.scope = ph12
    win_bf = P.sbuf("win_bf", [128, 8, 4640], BF16)
    bias3 = P.sbuf("bias3", [65, D], BF16)
    bxbc_t = P.sbuf("bxbc_t", [128, 12], F32)
    convw_t = P.sbuf("convw_t", [128, 12, 5], F32)
    convb_t = P.sbuf("convb_t", [128, 12], F32)
    dtb_bc = P.sbuf("dtb_bc", [128, 32], F32)
    a_bc = P.sbuf("a_bc", [128, 32], F32)

    ph0 = contextlib.ExitStack()
    P.scope = ph0
    cc_t = P.sbuf("cc_t", [128, 8], F32)
    cact = P.sbuf("cact", [128, 8], F32)
    badt = P.sbuf("badt", [128, 48], F32)
    wada = [P.sbuf("wada%d" % i, [128, 8, 512], F32) for i in range(2)]
    gb = P.sbuf("gb", [128, 8, 128], F32)
    tmp32 = P.sbuf("tmp32", [128, 64], F32)
    dma("sp", lambda e: e.dma_start(out=cc_t[:], in_=ccol[:, :]), "c4", writes=[cc_t])
    dma("sp", lambda e: e.dma_start(out=badt[:], in_=badac[:, :]), "c5", writes=[badt])
    op("act", lambda e: e.activation(out=cact[:], in_=cc_t[:], func=AF.Silu), reads=[cc_t], writes=[cact])
    wada_v = w_ada.rearrange("(k p) c -> p k c", p=128)
    b0, pm, pmk = pbank(1)
    for q in range(12):
        wt = wada[q % 2]
        dma("sp", lambda e, wt=wt, q=q: e.dma_start(out=wt[:], in_=wada_v[:, :, q * 512:(q + 1) * 512]),
            ("wada", q % 2), writes=[wt])
        for jj in range(4):
            def mm(e, wt=wt, q=q, jj=jj):
                for k in range(8):
                    ins = e.matmul(pm[:, q * 4 + jj:q * 4 + jj + 1], lhsT=wt[:, k, jj * 128:(jj + 1) * 128],
                                   rhs=cact[:, k:k + 1], start=(k == 0), stop=(k == 7))
                return ins
            op("pe", mm, reads=[wt, cact], writes=pmk)
    op("dve", lambda e: e.tensor_tensor(out=modc[:], in0=pm[:, 0:48], in1=badt[:], op=ALU.add),
       reads=pmk + [badt], writes=[modc])
    op("dve", lambda e: e.scalar_tensor_tensor(out=G12[:, 0:8], in0=modc[:, 8:16], scalar=1.0, in1=n12[:, 0:8],
                                               op0=ALU.add, op1=ALU.mult), reads=[modc, n12], writes=[G12])
    op("dve", lambda e: e.scalar_tensor_tensor(out=G12[:, 8:16], in0=modc[:, 32:40], scalar=1.0, in1=n12[:, 8:16],
                                               op0=ALU.add, op1=ALU.mult), reads=[modc, n12, G12], writes=[G12])

    def gate_bc(col0, dst, gbt):
        op("dve", lambda e: e.tensor_copy(out=gbt[:], in_=bcl(modc[:, col0:col0 + 8], [128, 8, 128])), reads=[modc], writes=[gbt])
        b_, pg, pgk = pbank(2)

        def mmg(e, pg=pg):
            for j in range(8):
                ins = e.matmul(pg[:, j * 128:(j + 1) * 128], lhsT=gbt[:, j, :], rhs=ident_f, start=True, stop=True)
            return ins
        op("pe", mmg, reads=[gbt, cf], writes=pgk)
        op("act", lambda e, pg=pg: e.activation(out=dst[:], in_=pg, func=AF.Copy), reads=pgk, writes=[dst])
    for k in range(8):
        dma("pool", lambda e, k=k: e.dma_start(out=win_bf[:, k, :], in_=w_in[k * 128:(k + 1) * 128, :]), "win", writes=[(win_bf, k)])
    for i in range(3):
        dma("pool", lambda e, i=i: e.dma_start(out=bias3[32 * i:32 * i + 1, :], in_=b_in[:, i * 1024:(i + 1) * 1024]), "binb", writes=[bias3])
    dma("sp", lambda e: e.dma_start(out=bxbc_t[:], in_=bxbc[:, :]), "c6", writes=[bxbc_t])
    dma("sp", lambda e: e.dma_start(out=convw_t[:], in_=convw[:, :, :]), "c7", writes=[convw_t])
    dma("sp", lambda e: e.dma_start(out=convb_t[:], in_=convb[:, :]), "c8", writes=[convb_t])
    dma("sp", lambda e: e.dma_start(out=tmp32[:, 0:64], in_=ssdv.partition_broadcast(128)), "c10", writes=[tmp32])
    dma("sp", lambda e: e.dma_start(out=dtb_bc[:], in_=b_in[:, 4608:4640].partition_broadcast(128)), "c11", writes=[dtb_bc])
    op("dve", lambda e: e.tensor_tensor(out=dtb_bc[:], in0=dtb_bc[:], in1=tmp32[:, 32:64], op=ALU.add),
       reads=[dtb_bc, tmp32], writes=[dtb_bc])
    op("act", lambda e: e.activation(out=a_bc[:], in_=tmp32[:, 0:32], func=AF.Exp), reads=[tmp32], writes=[a_bc])
    op("dve", lambda e: e.tensor_scalar(out=a_bc[:], in0=a_bc[:], scalar1=-1.0, scalar2=None, op0=ALU.mult),
       reads=[a_bc], writes=[a_bc])
    dump("modc", modc[:], [modc])
    P.flush()
    ph0.close()

    P.scope = ph12
    xin = [P.sbuf("xin%d" % i, [128, D], F32) for i in range(2)]
    xnb = [P.sbuf("xnb%d" % i, [128, D], BF16) for i in range(1)]
    hTs = [P.sbuf("hTt%d" % i, [128, 8, TL], BF16) for i in range(2)]
    h2c = P.sbuf("h2c", [128, 8, 2], BF16)
    st4 = [P.sbuf("st4_%d" % i, [128, 4], F32) for i in range(3)]
    pres = [P.sbuf("pre%d" % i, [128, 12, TL + 4], BF16) for i in range(2)]

    def curbuf():
        tb = getattr(P._tl, "tb", 0)
        return hTs[tb], pres[tb]
    hsave = P.sbuf("hsave", [128, 12, 2], BF16)
    dgall = P.sbuf("dgall", [128, 12, 5, 128], BF16)
    xs_tok = [P.sbuf("xs_tok%d" % i, [128, 16, 64], BF16) for i in range(2)]
    B_tok = [P.sbuf("B_tok%d" % i, [128, 256], BF16) for i in range(2)]
    dts = [P.sbuf("dts%d" % i, [128, 8, 32], F32) for i in range(2)]
    xw = [P.sbuf("xw%d" % i, [128, 16, 64], BF16) for i in range(1)]
    carry = P.sbuf("carry", [128, 16, 64], F32)
    carry_bf = P.sbuf("carrybf", [128, D], BF16)
    gt1 = P.sbuf("gt1", [128, D], F32)
    ctmp = gt1[:].rearrange("p (h d) -> p h d", d=64)
    cnt = {"x": 0, "xn": 0, "st": 0, "ca": 0, "ch": 0, "dg": 0}

    for j in range(12):
        for kk in range(5):
            op("dve", lambda e, j=j, kk=kk: e.tensor_scalar(out=dgall[:, j, kk, :], in0=ident_f, scalar1=convw_t[:, j, kk:kk + 1],
                                                           scalar2=None, op0=ALU.mult), reads=[cf, convw_t], writes=[dgall])

    def rot(lst, key):
        i = cnt[key]
        cnt[key] += 1
        return lst[i % len(lst)]

    def rstd_ops(ss_ap, out_ap, n, keys):
        op("act", lambda e: e.activation(out=out_ap, in_=ss_ap, func=AF.Ln, bias=EPS, scale=1.0 / n), reads=keys, writes=keys)
        op("act", lambda e: e.activation(out=out_ap, in_=out_ap, func=AF.Exp, scale=-0.5), reads=keys, writes=keys)

    def norm_T(src, r0, np_, Gc, SHc, dst_fn, dkeys):
        xt = rot(xin, "x")
        dma("sp", lambda e: e.dma_start(out=xt[0:np_, :], in_=src[r0:r0 + np_, :]), xt, writes=[xt])
        s4 = rot(st4, "st")
        op("act", lambda e: e.activation(out=junk[0:np_, :], in_=xt[0:np_, :], func=AF.Square, accum_out=s4[0:np_, 0:1]),
           reads=[xt], writes=[s4, "junk"])
        rstd_ops(s4[0:np_, 0:1], s4[0:np_, 1:2], D, [s4])
        xn = rot(xnb, "xn")
        op("dve", lambda e: e.tensor_scalar(out=xn[0:np_, :], in0=xt[0:np_, :], scalar1=s4[0:np_, 1:2], scalar2=None,
                                            op0=ALU.mult), reads=[xt, s4], writes=[xn])
        b_, pt, ptk = pbank(1)
        ptb = pt.bitcast(BF16)

        def tr(e):
            for k in range(8):
                ins = e.transpose(out=ptb[:, k * np_:(k + 1) * np_], in_=xn[0:np_, k * 128:(k + 1) * 128],
                                  identity=ident_bf[0:np_, 0:np_])
            return ins
        op("pe", tr, reads=[xn, cbf], writes=ptk)
        dst = dst_fn(None)
        op("dve", lambda e: e.tensor_tensor(out=dst, in0=ptb[:, 0:8 * np_].rearrange("p (k t) -> p k t", t=np_),
                                            in1=bcl(Gc, [128, 8, np_]), op=ALU.mult), reads=ptk + [G12], writes=dkeys)
        op("dve", lambda e: e.tensor_tensor(out=dst, in0=dst, in1=bcl(SHc, [128, 8, np_]), op=ALU.add),
           reads=dkeys + [modc], writes=dkeys)

    def build_hT(t, src, Gc, SHc):
        hTt, pre = curbuf()
        for c4 in range(CPT):
            norm_T(src, (t * CPT + c4) * 128, 128, Gc, SHc, lambda k, c4=c4: hTt[:, :, c4 * 128:(c4 + 1) * 128], [(hTt, c4)])

    def proj_fm(t, blocks, reverse):
        hTt, pre = curbuf()
        tn = t - 1 if reverse else t + 1
        tp = t + 1 if reverse else t - 1
        far = slice(0, 2) if reverse else slice(TL + 2, TL + 4)
        near = slice(TL + 2, TL + 4) if reverse else slice(0, 2)
        src_near = slice(2, 4) if reverse else slice(TL, TL + 2)
        nb_ = len(blocks)
        PK = [(pre, j) for j in blocks]
        if 0 <= tn < NT:
            rr = t * TL - 2 if reverse else (t + 1) * TL
            norm_T(x, rr, 2, G12[:, 0:8], modc[:, 0:8], lambda k: h2c[:, :, :], [h2c])
        for j in blocks:
            c0 = 3072 + j * 128
            b_, pp, ppk = pbank(1)

            def mm(e, pp=pp, c0=c0):
                for k in range(8):
                    ins = e.matmul(pp[:, 0:TL], lhsT=win_bf[:, k, c0:c0 + 128], rhs=hTt[:, k, :], start=(k == 0), stop=(k == 7))
                return ins
            op("pe", mm, reads=[(win_bf, k) for k in range(8)] + [(hTt, c) for c in range(CPT)], writes=ppk)
            op("act", lambda e, pp=pp, j=j: e.activation(out=pre[:, j, 2:2 + TL], in_=pp[:, 0:TL], func=AF.Identity,
                                                        bias=bxbc_t[:, j:j + 1], scale=1.0),
               reads=ppk + [bxbc_t], writes=[(pre, j)])
        if 0 <= tn < NT:
            b_, ph_, phk = pbank(1)

            def mmh(e, ph_=ph_):
                for j in blocks:
                    c0 = 3072 + j * 128
                    for k in range(8):
                        ins = e.matmul(ph_[:, j * 2:j * 2 + 2], lhsT=win_bf[:, k, c0:c0 + 128], rhs=h2c[:, k, :],
                                       start=(k == 0), stop=(k == 7))
                return ins
            op("pe", mmh, reads=[(win_bf, k) for k in range(8)] + [h2c], writes=phk)
            op("dve", lambda e, ph_=ph_: e.tensor_tensor(
                out=pre[:, 0:nb_, far], in0=ph_[:, 0:2 * nb_].rearrange("p (j c) -> p j c", c=2),
                in1=bcl(bxbc_t[:, 0:nb_], [128, nb_, 2]), op=ALU.add),
                reads=phk + [bxbc_t], writes=PK)
        else:
            op("dve", lambda e: e.memset(pre[:, 0:nb_, far], 0.0), writes=PK)
        if 0 <= tp < NT:
            op("dve", lambda e: e.tensor_copy(out=pre[:, 0:nb_, near], in_=hsave[:, 0:nb_, :]), reads=[hsave], writes=PK)
        else:
            op("dve", lambda e: e.memset(pre[:, 0:nb_, near], 0.0), writes=PK)
        op("dve", lambda e: e.tensor_copy(out=hsave[:, 0:nb_, :], in_=pre[:, 0:nb_, src_near]), reads=PK, writes=[hsave])

    def conv_silu(t, blocks):
        hTt, pre = curbuf()
        for j in blocks:
            b_, pcv, pcvk = pbank(1)

            def mmc(e, j=j, pcv=pcv):
                for kk in range(5):
                    ins = e.matmul(pcv[:, 0:TL], lhsT=dgall[:, j, kk, :], rhs=pre[:, j, kk:kk + TL], start=(kk == 0), stop=(kk == 4))
                return ins
            op("pe", mmc, reads=[dgall, (pre, j)], writes=pcvk)
            op("act", lambda e, pcv=pcv, j=j: e.activation(out=pre[:, j, 2:2 + TL], in_=pcv[:, 0:TL], func=AF.Silu,
                                                          bias=convb_t[:, j:j + 1], scale=1.0),
               reads=pcvk + [convb_t], writes=[(pre, j)])

    def to_tok(c4, par):
        hTt, pre = curbuf()
        b_, pt, ptk = pbank(1)
        ptb = pt.bitcast(BF16)

        def tr(e, ptb=ptb):
            for j in range(8):
                ins = e.transpose(out=ptb[:, j * 128:(j + 1) * 128], in_=pre[:, j, 2 + c4 * 128:2 + (c4 + 1) * 128], identity=ident_bf)
            return ins
        op("pe", tr, reads=[(pre, j) for j in range(8)] + [cbf], writes=ptk)
        op("act", lambda e, ptb=ptb: e.activation(out=xs_tok[par][:].rearrange("p h d -> p (h d)"), in_=ptb, func=AF.Copy),
           reads=ptk, writes=[xs_tok[par]])
        b2, pt2, pt2k = pbank(1)
        pt2b = pt2.bitcast(BF16)

        def tr2(e, pt2b=pt2b):
            for g in range(2):
                ins = e.transpose(out=pt2b[:, g * 128:(g + 1) * 128], in_=pre[:, 8 + g, 2 + c4 * 128:2 + (c4 + 1) * 128], identity=ident_bf)
            return ins
        op("pe", tr2, reads=[(pre, 8), (pre, 9), cbf], writes=pt2k)
        op("dve", lambda e, pt2b=pt2b: e.tensor_copy(out=B_tok[par][:], in_=pt2b[:, 0:256]), reads=pt2k, writes=[B_tok[par]])

    def dt_proj(c4, dd):
        hTt, pre = curbuf()
        b_, pd, pdk = pbank(1)

        def mm2(e, pd=pd):
            for k in range(8):
                ins = e.matmul(pd[:, 0:32], lhsT=hTt[:, k, c4 * 128:(c4 + 1) * 128], rhs=win_bf[:, k, 4608:4640],
                               start=(k == 0), stop=(k == 7))
            return ins
        op("pe", mm2, reads=[(win_bf, k) for k in range(8)] + [(hTt, c4)], writes=pdk)
        v, m, na = dd[:, 2, :], dd[:, 3, :], dd[:, 4, :]
        op("dve", lambda e: e.tensor_tensor(out=v, in0=pd[:, 0:32], in1=dtb_bc[:], op=ALU.add), reads=pdk + [dtb_bc], writes=[dd])
        op("dve", lambda e: e.tensor_scalar(out=m, in0=v, scalar1=0.0, scalar2=None, op0=ALU.max), reads=[dd], writes=[dd])
        op("dve", lambda e: e.scalar_tensor_tensor(out=na, in0=m, scalar=-2.0, in1=v, op0=ALU.mult, op1=ALU.add), reads=[dd], writes=[dd])
        op("act", lambda e: e.activation(out=na, in_=na, func=AF.Exp), reads=[dd], writes=[dd])
        op("act", lambda e: e.activation(out=na, in_=na, func=AF.Ln, bias=1.0, scale=1.0), reads=[dd], writes=[dd])
        op("dve", lambda e: e.tensor_tensor(out=dd[:, 0, :], in0=m, in1=na, op=ALU.add), reads=[dd], writes=[dd])
        op("dve", lambda e: e.tensor_tensor(out=dd[:, 1, :], in0=dd[:, 0, :], in1=a_bc[:], op=ALU.mult), reads=[dd, a_bc], writes=[dd])

    def state_update(dd, hs, tri_f, first, par=0):
        b_, pq, pqk = pbank(1)
        op("pe", lambda e: e.matmul(pq[:, 0:16], lhsT=tri_f, rhs=dd[:, 1, hs], start=True, stop=True), reads=[dd, cf], writes=pqk)
        op("pe", lambda e: e.matmul(pq[:, 16:32], lhsT=ones_f, rhs=dd[:, 1, hs], start=True, stop=True), reads=[dd, cf] + pqk, writes=pqk)
        wv, dec = dd[:, 5, 0:16], dd[:, 5, 16:32]
        op("act", lambda e: e.activation(out=dd[:, 5, :], in_=pq[:, 0:32], func=AF.Exp), reads=pqk, writes=[dd])
        op("dve", lambda e: e.tensor_tensor(out=wv, in0=wv, in1=dd[:, 0, hs], op=ALU.mult), reads=[dd], writes=[dd])
        xwt = xw[0]
        op("dve", lambda e: e.tensor_tensor(out=xwt[:], in0=xs_tok[par][:], in1=bcl(wv, [128, 16, 64]), op=ALU.mult),
           reads=[xs_tok[par], dd], writes=[xwt])
        b2, psS, psk = pbank(2)

        def mm(e):
            for g in range(2):
                ins = e.matmul(psS[:, g * 512:(g + 1) * 512], lhsT=B_tok[par][:, g * 128:(g + 1) * 128],
                               rhs=xwt[:, g * 8:(g + 1) * 8, :].rearrange("p h d -> p (h d)"), start=True, stop=True)
            return ins
        op("pe", mm, reads=[B_tok[par], xwt], writes=psk)
        crf = carry[:].rearrange("p h d -> p (h d)")
        if first:
            op("dve", lambda e: e.tensor_copy(out=crf, in_=psS), reads=psk, writes=[carry])
        else:
            op("dve", lambda e: e.tensor_tensor(out=carry[:], in0=carry[:], in1=bcl(dec, [128, 16, 64]), op=ALU.mult),
               reads=[carry, dd], writes=[carry])
            op("dve", lambda e: e.tensor_tensor(out=crf, in0=crf, in1=psS, op=ALU.add),
               reads=[carry] + psk, writes=[carry])
        op("act", lambda e: e.activation(out=carry_bf[:], in_=crf, func=AF.Copy), reads=[carry], writes=[carry_bf])

    ph1x = contextlib.ExitStack()
    P.scope = ph1x
    wstg = [P.sbuf("wstg%d" % i, [128, 4096], BF16) for i in range(3)]
    P.scope = ph12
    wsrc = [(w13p.rearrange("(n p r) c -> n p (r c)", p=128, r=2), w13c_d.rearrange("(n p r) c -> n p (r c)", p=128, r=2), 32),
            (w2p.rearrange("(n p r) c -> n p (r c)", p=128, r=2), w2c_d.rearrange("(n p r) c -> n p (r c)", p=128, r=2), 16)]
    wjobs = [(sv, dv, n) for (sv, dv, cnt_) in wsrc for n in range(cnt_)]
    wpos = {"ld": 0, "st": 0}

    def wconv_step(nsteps):
        for _ in range(nsteps):
            if wpos["ld"] < len(wjobs):
                sv, dv, n = wjobs[wpos["ld"]]
                wb = wstg[wpos["ld"] % 3]
                dma("pool", lambda e, sv=sv, n=n, wb=wb: e.dma_start(out=wb[:], in_=sv[n]), wb, writes=[wb])
                wpos["ld"] += 1
            if wpos["ld"] - wpos["st"] >= 2 or (wpos["ld"] == len(wjobs) and wpos["st"] < len(wjobs)):
                sv, dv, n = wjobs[wpos["st"]]
                wb = wstg[wpos["st"] % 3]
                dma("pool", lambda e, dv=dv, n=n, wb=wb: e.dma_start(out=dv[n], in_=wb[:]), ("wst", wpos["st"] % 3), reads=[wb])
                wpos["st"] += 1
    XSB = list(range(10))
    op("dve", lambda e: e.memset(carry_bf[:], 0.0), writes=[carry_bf])

    def prep1(t):
        P._tl.tb = t % 2
        build_hT(t, x, G12[:, 0:8], modc[:, 0:8])
        proj_fm(t, XSB, True)
        wconv_step(2)
        conv_silu(t, XSB)
        wconv_step(1)

    def chunks1(t):
        P._tl.tb = t % 2
        for c4 in range(CPT - 1, -1, -1):
            c = t * CPT + c4
            dd = dts[c % 2]
            dma("sp", lambda e, c=c: e.dma_start(out=prevb_d[c, :, :], in_=carry_bf[:]), "prevb_st", reads=[carry_bf])
            to_tok(c4, 0)
            dt_proj(c4, dd)
            state_update(dd, slice(16, 32), SL_f, first=(c == NCH - 1))
    prep1(NT - 1)
    for t in range(NT - 1, -1, -1):
        fns = [lambda t=t: chunks1(t)]
        qs = [3]
        if t > 0:
            fns.append(lambda t=t: prep1(t - 1))
            qs.append(4)
        P.interleave(fns, qs)
    P._tl.tb = 0
    wconv_step(len(wjobs) + 2)
    assert wpos["st"] == len(wjobs)
    P.flush()
    ph1x.close()

    ph2 = contextlib.ExitStack()
    P.scope = ph2
    lng_bc = P.sbuf("lng_bc", [128, D], BF16)
    CB = P.sbuf("CB", [128, 8, 128], F32)
    wsT_bf = P.sbuf("wsT_bf", [128, 8, 128], BF16)
    rsw = P.sbuf("rsw", [128, 16], F32)
    Ddiag = P.sbuf("Ddiag", [128, 8, 128], BF16)
    dsk_t = P.sbuf("dsk_t", [128, 8], F32)
    SGt = P.sbuf("SGt", [128, 128], F32)
    bs_t = P.sbuf("bs_t", [128, 8], F32)
    guv = P.sbuf("guv", [128, 2 * D], BF16)
    vnb = P.sbuf("vnb", [128, D], BF16)
    yab = vnb
    yabS = P.sbuf("yabS", [128, D], BF16)
    hib = P.sbuf("hib", [128, 32], BF16)
    yTa = [P.sbuf("yTa%d" % i, [128, 8, 128], BF16) for i in range(2)]
    yTb = P.sbuf("yTb", [128, 8, 128], BF16)
    zss = [P.sbuf("zs%d" % i, [128, D], BF16) for i in range(2)]
    Ef = P.sbuf("Ef", [128, 16, 128], BF16)
    Eb = P.sbuf("Eb", [128, 16, 128], BF16)
    scT = P.sbuf("scT", [128, 2, 128], BF16)
    pvb = [P.sbuf("pvb%d" % i, [128, D], BF16) for i in range(1)]
    y1 = P.sbuf("y1", [128, 16, 64], F32)
    y1b = y1[:].rearrange("p h d -> p (h d)").bitcast(BF16)
    rhsb = [y1b[:, i * D:(i + 1) * D].rearrange("p (h l) -> p h l", l=128) for i in range(2)]
    rhsb2 = [xw[0][:].rearrange("p h d -> p (h d)").rearrange("p (h l) -> p h l", l=128),
             yabS[:].rearrange("p (h l) -> p h l", l=128)]
    RB = [(rhsb, [(y1, 0), (y1, 1)]), (rhsb2, [xw[0], yabS])]
    sm = [P.sbuf("sm%d" % i, [128, 16], F32) for i in range(2)]
    wtmp2 = y1[:].rearrange("p h d -> p (h d)").rearrange("p (h d) -> p h d", d=128)
    lnb_bc = gt1
    dma("pool", lambda e: e.dma_start(out=lng_bc[:], in_=lngb[0:1, :].partition_broadcast(128)), "p2a", writes=[lng_bc])
    dma("sp", lambda e: e.dma_start(out=lnb_bc[:], in_=lngb[1:2, :].partition_broadcast(128)), "p2b", writes=[gt1])
    dma("sp", lambda e: e.dma_start(out=wtmp2, in_=w_sT[:, :, :]), "p2c", writes=[y1])
    dma("sp", lambda e: e.dma_start(out=bs_t[:], in_=b_s2[:, :]), "p2d", writes=[bs_t])
    dma("sp", lambda e: e.dma_start(out=dsk_t[:], in_=dskc[:, :]), "p2e", writes=[dsk_t])
    op("act", lambda e: e.activation(out=wsT_bf[:], in_=wtmp2, func=AF.Copy), reads=[y1], writes=[wsT_bf])
    b_, prs, prsk = pbank(1)

    def mmrs(e):
        for h in range(8):
            ins = e.matmul(prs[:, h:h + 1], lhsT=wtmp2[:, h, :], rhs=ones_f[:, 0:1], start=True, stop=True)
        return ins
    op("pe", mmrs, reads=[y1, cf], writes=prsk)
    op("dve", lambda e: e.tensor_copy(out=rsw[:, 0:8], in_=prs[:, 0:8]), reads=prsk, writes=[rsw])
    op("dve", lambda e: e.tensor_tensor(out=CB[:], in0=lnb_bc[:].rearrange("p (h d) -> p h d", d=128),
                                        in1=bcl(rsw[:, 0:8], [128, 8, 128]), op=ALU.mult), reads=[gt1, rsw], writes=[CB])
    op("dve", lambda e: e.tensor_tensor(out=CB[:], in0=CB[:], in1=bcl(bs_t[:], [128, 8, 128]), op=ALU.add),
       reads=[CB, bs_t], writes=[CB])
    for j in range(8):
        op("dve", lambda e, j=j: e.tensor_scalar(out=Ddiag[:, j, :], in0=ident_f, scalar1=dsk_t[:, j:j + 1], scalar2=None,
                                                op0=ALU.mult), reads=[cf, dsk_t], writes=[Ddiag])
    SG_f = SGt[:]
    op("dve", lambda e: e.tensor_tensor(out=SGt[:], in0=L_f, in1=ident_f, op=ALU.subtract), reads=[cf], writes=[SGt])
    P.flush()
    ALLB = list(range(12))
    wink = [(win_bf, k) for k in range(8)]

    def tok_proj(c4, col0, nblk, pz):
        hTt, pre = curbuf()
        def mm(e):
            for k in range(8):
                for n in range(nblk):
                    c0 = col0 + n * 512
                    e.matmul(pz[:, n * 512:(n + 1) * 512], lhsT=hTt[:, k, c4 * 128:(c4 + 1) * 128],
                             rhs=win_bf[:, k, c0:c0 + 512], start=(k == 0), stop=False)
            for n in range(nblk):
                c0 = col0 + n * 512
                r = 32 * (c0 // 1024)
                ins = e.matmul(pz[:, n * 512:(n + 1) * 512], lhsT=ones_bf[r:r + 1, :], rhs=bias3[r:r + 1, c0 % 1024:c0 % 1024 + 512],
                               start=False, stop=True)
            return ins
        return mm

    def gmlp_chunk(c4, ya):
        hTt, pre = curbuf()
        g = guv
        s = sm[0]
        for half in range(2):
            b_, pu, puk = pbank(2)
            op("pe", tok_proj(c4, half * 1024, 2, pu), reads=wink + [(hTt, c4), bias3, cbf], writes=puk)
            if half == 0:
                op("act", lambda e, pu=pu: e.activation(out=g[:, 0:1024], in_=pu, func=AF.Gelu), reads=puk, writes=[(g, 0)])
            else:
                op("act", lambda e, pu=pu: e.activation(out=g[:, 1024:2048], in_=pu, func=AF.Gelu, accum_out=s[:, 0:1]),
                   reads=puk, writes=[(g, 1), s])
        v = g[:, 1024:2048]
        op("act", lambda e: e.activation(out=junk[:], in_=v, func=AF.Square, accum_out=s[:, 1:2]), reads=[(g, 1), s], writes=[s, "junk"])
        op("dve", lambda e: e.tensor_scalar(out=s[:, 2:3], in0=s[:, 0:1], scalar1=1.0 / 1024, scalar2=None, op0=ALU.mult), reads=[s], writes=[s])
        op("dve", lambda e: e.tensor_tensor(out=s[:, 3:4], in0=s[:, 2:3], in1=s[:, 2:3], op=ALU.mult), reads=[s], writes=[s])
        op("dve", lambda e: e.scalar_tensor_tensor(out=s[:, 4:5], in0=s[:, 1:2], scalar=1.0 / 1024, in1=s[:, 3:4],
                                                   op0=ALU.mult, op1=ALU.subtract), reads=[s], writes=[s])
        op("act", lambda e: e.activation(out=s[:, 5:6], in_=s[:, 4:5], func=AF.Ln, bias=EPS, scale=1.0), reads=[s], writes=[s])
        op("act", lambda e: e.activation(out=s[:, 5:6], in_=s[:, 5:6], func=AF.Exp, scale=-0.5), reads=[s], writes=[s])
        op("dve", lambda e: e.scalar_tensor_tensor(out=s[:, 6:7], in0=s[:, 2:3], scalar=-1.0, in1=s[:, 5:6],
                                                   op0=ALU.mult, op1=ALU.mult), reads=[s], writes=[s])
        op("dve", lambda e: e.tensor_scalar(out=vnb[:], in0=v, scalar1=s[:, 5:6], scalar2=s[:, 6:7], op0=ALU.mult, op1=ALU.add),
           reads=[(g, 1), s], writes=[vnb])
        b_, pmx, pmxk = pbank(2)

        def mms(e, pmx=pmx):
            for hh in range(8):
                ins = e.matmul(pmx[:, hh * 128:(hh + 1) * 128], lhsT=wsT_bf[:, hh, :], rhs=vnb[:, hh * 128:(hh + 1) * 128],
                               start=True, stop=True)
            return ins
        op("pe", mms, reads=[wsT_bf, vnb], writes=pmxk)
        op("dve", lambda e, pmx=pmx: e.tensor_tensor(out=gt1[:], in0=pmx, in1=lng_bc[:], op=ALU.mult), reads=pmxk + [lng_bc], writes=[gt1])
        op("dve", lambda e: e.tensor_tensor(out=gt1[:], in0=gt1[:], in1=CB[:].rearrange("p h d -> p (h d)"), op=ALU.add),
           reads=[gt1, CB], writes=[gt1])
        op("dve", lambda e: e.tensor_tensor(out=gt1[:], in0=gt1[:], in1=g[:, 0:1024], op=ALU.mult), reads=[gt1, (g, 0)], writes=[gt1])
        op("act", lambda e: e.activation(out=junk[:], in_=gt1[:], func=AF.Square, accum_out=s[:, 7:8]), reads=[gt1, s], writes=[s, "junk"])
        rstd_ops(s[:, 7:8], s[:, 8:9], D, [s])
        op("act", lambda e: e.activation(out=yab[:], in_=gt1[:], func=AF.Copy, scale=s[:, 8:9]), reads=[gt1, s], writes=[yab])
        b_, pt, ptk = pbank(1)
        ptb = pt.bitcast(BF16)

        def tr(e, ptb=ptb):
            for k in range(8):
                ins = e.transpose(out=ptb[:, k * 128:(k + 1) * 128], in_=yab[:, k * 128:(k + 1) * 128], identity=ident_bf)
            return ins
        op("pe", tr, reads=[yab, cbf], writes=ptk)
        op("dve", lambda e, ptb=ptb: e.tensor_copy(out=yTa[ya][:].rearrange("p k t -> p (k t)"), in_=ptb), reads=ptk, writes=[yTa[ya]])

    def z_chunk(c4, par):
        hTt, pre = curbuf()
        zs = zss[par]
        b_, pz, pzk = pbank(2)
        op("pe", tok_proj(c4, 2048, 2, pz), reads=wink + [(hTt, c4), bias3, cbf], writes=pzk)
        op("act", lambda e, pz=pz: e.activation(out=zs[:], in_=pz, func=AF.Silu), reads=pzk, writes=[zs])

    def decay_prep(dd):
        b_, pc, pck = pbank(1)
        op("pe", lambda e: e.matmul(pc[:, 0:16], lhsT=U_f, rhs=dd[:, 1, 0:16], start=True, stop=True), reads=[dd, cf], writes=pck)
        op("pe", lambda e: e.matmul(pc[:, 16:32], lhsT=L_f, rhs=dd[:, 1, 16:32], start=True, stop=True), reads=[dd, cf] + pck, writes=pck)
        op("dve", lambda e: e.tensor_copy(out=dd[:, 6, :], in_=pc[:, 0:32]), reads=pck, writes=[dd])
        op("act", lambda e: e.activation(out=dd[:, 7, :], in_=dd[:, 0, :], func=AF.Ln), reads=[dd], writes=[dd])
        op("dve", lambda e: e.tensor_tensor(out=dd[:, 7, :], in0=dd[:, 7, :], in1=dd[:, 6, :], op=ALU.subtract), reads=[dd], writes=[dd])
        op("dve", lambda e: e.tensor_copy(out=hib[:], in_=dd[:, 1, :]), reads=[dd], writes=[hib])
        op("dve", lambda e: e.tensor_copy(out=dd[:, 2, :], in_=hib[:]), reads=[hib], writes=[dd])
        op("dve", lambda e: e.tensor_tensor(out=dd[:, 3, :], in0=dd[:, 1, :], in1=dd[:, 2, :], op=ALU.subtract), reads=[dd], writes=[dd])

    def decay_stages(dd):
        stage = 0
        for q in range(2):
            for (h0, tri_bf, mask_bf, Eout) in ((0, U_bf, mF_bf, Ef), (16, L_bf, mB_bf, Eb)):
                rb, rbk = RB[stage % 2]
                stage += 1
                for i in range(2):
                    op("dve", lambda e, i=i, q=q, h0=h0, tri_bf=tri_bf, rb=rb: e.tensor_tensor(
                        out=rb[i], in0=bcm(tri_bf, [128, 8, 128]),
                        in1=bcl(dd[:, 2 + i, h0 + q * 8:h0 + (q + 1) * 8], [128, 8, 128]), op=ALU.mult),
                       reads=[dd, cbf], writes=[rbk[i]])
                b_, pa, pak = pbank(2)

                def mm(e, pa=pa, mask_bf=mask_bf, rb=rb):
                    for n in range(2):
                        hh = n * 4
                        o_ = pa[:, n * 512:(n + 1) * 512]
                        e.matmul(o_, lhsT=ones_bf, rhs=rb[0][:, hh:hh + 4, :].rearrange("p h l -> p (h l)"), start=True, stop=False)
                        e.matmul(o_, lhsT=ones_bf, rhs=rb[1][:, hh:hh + 4, :].rearrange("p h l -> p (h l)"), start=False, stop=False)
                        for h4 in range(4):
                            ins = e.matmul(o_[:, h4 * 128:(h4 + 1) * 128], lhsT=ident_bf, rhs=mask_bf, start=False, stop=(h4 == 3))
                    return ins
                op("pe", mm, reads=rbk + [cbf], writes=pak)
                for h8 in range(8):
                    hh = q * 8 + h8
                    hsel = h0 + hh
                    op("act", lambda e, pa=pa, h8=h8, hh=hh, hsel=hsel, Eout=Eout: e.activation(
                        out=Eout[:, hh, :], in_=pa[:, h8 * 128:(h8 + 1) * 128], func=AF.Exp, bias=dd[:, 7, hsel:hsel + 1], scale=1.0),
                        reads=pak + [dd], writes=[(Eout, hh)])

    def ssd_chunk(t, c4, c, par):
        hTt, pre = curbuf()
        dd = dts[par]
        zs = zss[par]
        pv = pvb[0]
        dma("sp", lambda e, pv=pv, c=c: e.dma_start(out=pv[:], in_=prevb_d[c, :, :]), pv, writes=[pv])
        decay_stages(dd)
        EFK = [(Ef, h) for h in range(16)]
        EBK = [(Eb, h) for h in range(16)]
        op("dve", lambda e: e.tensor_tensor(out=Ef[:], in0=Ef[:], in1=Eb[:], op=ALU.add), reads=EFK + EBK, writes=EFK)
        b_, psc, psck = pbank(1)

        def mmsc(e, psc=psc):
            for g in range(2):
                ins = e.matmul(psc[:, g * 128:(g + 1) * 128], lhsT=pre[:, 8 + g, 2 + c4 * 128:2 + (c4 + 1) * 128],
                               rhs=pre[:, 10 + g, 2 + c4 * 128:2 + (c4 + 1) * 128], start=True, stop=True)
            return ins
        op("pe", mmsc, reads=[(pre, j) for j in range(8, 12)], writes=psck)
        op("act", lambda e, psc=psc: e.activation(out=scT[:].rearrange("p g l -> p (g l)"), in_=psc[:, 0:256], func=AF.Copy),
           reads=psck, writes=[scT])
        Mt = Eb
        for g in range(2):
            op("dve", lambda e, g=g: e.tensor_tensor(
                out=Mt[:, g * 8:(g + 1) * 8, :], in0=Ef[:, g * 8:(g + 1) * 8, :], in1=bcm(scT[:, g, :], [128, 8, 128]), op=ALU.mult),
               reads=EFK + [scT], writes=[(Eb, h) for h in range(g * 8, g * 8 + 8)])
        xst = xs_tok[par]
        Y1K = [(y1, 0), (y1, 1)]
        CT = [pre[:, 10 + g, 2 + c4 * 128:2 + (c4 + 1) * 128] for g in range(2)]
        op("act", lambda e: e.activation(out=dd[:, 5, :], in_=dd[:, 6, :], func=AF.Exp), reads=[dd], writes=[dd])
        yv = y1[:].rearrange("p h d -> p (h d)")

        def yoff(st):
            b_, po_, pok = pbank(2)

            def mmo(e, po_=po_, st=st):
                for g in range(2):
                    ins = e.matmul(po_[:, g * 512:(g + 1) * 512], lhsT=CT[g], rhs=st[:, g * 512:(g + 1) * 512], start=True, stop=True)
                return ins
            op("pe", mmo, reads=[(pre, 10), (pre, 11), st], writes=pok)
            return po_, pok
        have_f = c > 0
        have_b = c < NCH - 1
        if have_f:
            po_, pok = yoff(carry_bf)
            op("dve", lambda e, po_=po_: e.tensor_tensor(out=y1[:], in0=po_.rearrange("p (h d) -> p h d", d=64),
                                                         in1=bcl(dd[:, 5, 0:16], [128, 16, 64]), op=ALU.mult), reads=pok + [dd], writes=Y1K)
        b_, py, pyk = pbank(2)

        def mmy(e, py=py):
            for j in range(8):
                for hh in range(2):
                    h = 2 * j + hh
                    e.matmul(py[:, h * 64:(h + 1) * 64], lhsT=pre[:, j, 2 + c4 * 128:2 + (c4 + 1) * 128],
                             rhs=Ddiag[:, j, hh * 64:(hh + 1) * 64], start=True, stop=False)
                    ins = e.matmul(py[:, h * 64:(h + 1) * 64], lhsT=Mt[:, h, :], rhs=xst[:, h, :], start=False, stop=True)
            return ins
        op("pe", mmy, reads=[(pre, j) for j in range(8)] + [Ddiag, xst] + EBK, writes=pyk)
        if have_f:
            op("dve", lambda e, py=py: e.tensor_tensor(out=yv, in0=py, in1=yv, op=ALU.add), reads=pyk + Y1K, writes=Y1K)
        else:
            op("dve", lambda e, py=py: e.tensor_copy(out=yv, in_=py), reads=pyk, writes=Y1K)
        if have_b:
            po_, pok = yoff(pv)
            op("dve", lambda e, po_=po_: e.tensor_tensor(out=po_.rearrange("p (h d) -> p h d", d=64), in0=po_.rearrange("p (h d) -> p h d", d=64),
                                                         in1=bcl(dd[:, 5, 16:32], [128, 16, 64]), op=ALU.mult), reads=pok + [dd], writes=pok)
            op("dve", lambda e, po_=po_: e.tensor_tensor(out=yv, in0=po_, in1=yv, op=ALU.add), reads=pok + Y1K, writes=Y1K)
        op("dve", lambda e: e.tensor_tensor(out=yv, in0=yv, in1=zs[:], op=ALU.mult), reads=Y1K + [zs], writes=Y1K)
        s = sm[1]
        for g in range(2):
            op("act", lambda e, g=g: e.activation(out=junk[:, 0:512], in_=yv[:, g * 512:(g + 1) * 512], func=AF.Square,
                                                  accum_out=s[:, g:g + 1]), reads=Y1K + [s], writes=[s, "junk"])
        rstd_ops(s[:, 0:2], s[:, 2:4], 512, [s])
        for g in range(2):
            op("act", lambda e, g=g: e.activation(out=yabS[:, g * 512:(g + 1) * 512], in_=yv[:, g * 512:(g + 1) * 512], func=AF.Copy,
                                                  scale=s[:, 2 + g:3 + g]), reads=Y1K + [s], writes=[yabS])
        b_, pt, ptk = pbank(1)
        ptb = pt.bitcast(BF16)

        def tr(e, ptb=ptb):
            for k in range(8):
                ins = e.transpose(out=ptb[:, k * 128:(k + 1) * 128], in_=yabS[:, k * 128:(k + 1) * 128], identity=ident_bf)
            return ins
        op("pe", tr, reads=[yabS, cbf], writes=ptk)
        op("dve", lambda e, ptb=ptb: e.tensor_copy(out=yTb[:].rearrange("p k t -> p (k t)"), in_=ptb), reads=ptk, writes=[yTb])
        if c < NCH - 1:
            state_update(dd, slice(0, 16), SG_f, first=(c == 0), par=par)

    def outproj_chunk(t, c4, ya):
        c = t * CPT + c4
        dma("sp", lambda e: e.dma_start(out=yT_d[c, :, 0:8, :], in_=yTa[ya][:]), ("yTa_st", ya), reads=[yTa[ya]])
        dma("sp", lambda e: e.dma_start(out=yT_d[c, :, 8:16, :], in_=yTb[:]), "yTb_st", reads=[yTb])

    def prep_tile(t):
        P._tl.tb = t % 2
        build_hT(t, x, G12[:, 0:8], modc[:, 0:8])
        proj_fm(t, ALLB, False)
        conv_silu(t, ALLB)

    def S_stream(c):
        t, c4 = divmod(c, CPT)
        P._tl.tb = t % 2
        ssd_chunk(t, c4, c, c % 2)
        outproj_chunk(t, c4, c % 2)

    def G_stream(c):
        t, c4 = divmod(c, CPT)
        P._tl.tb = t % 2
        par = c % 2
        z_chunk(c4, par)
        to_tok(c4, par)
        dt_proj(c4, dts[par])
        decay_prep(dts[par])
        gmlp_chunk(c4, par)
    prep_tile(0)
    G_stream(0)
    for c in range(NCH):
        t, c4 = divmod(c, CPT)
        fns = [lambda c=c: S_stream(c)]
        qs = [5]
        if c + 1 < NCH:
            fns.append(lambda c=c: G_stream(c + 1))
            qs.append(3)
        else:
            fns.append(lambda: None)
            qs.append(1)
        if c4 == 0 and t + 1 < NT:
            fns.append(lambda t=t: prep_tile(t + 1))
            qs.append(4)
        P.interleave(fns, qs)
    P._tl.tb = 0
    P.flush()
    ph2.close()
    ph12.close()

    ph3 = contextlib.ExitStack()
    P.scope = ph3
    TSL = 256
    NTL = (2 * S) // TSL + 32
    NSLOT = NTL * TSL
    hs_d = nc.dram_tensor("hs_s", [NSLOT, D], BF16, kind=skind).ap()
    ys_d = nc.dram_tensor("ys_s", [NSLOT, D], BF16, kind=skind).ap()
    psn[0] = 0
    gate2_bc = P.sbuf("gate2_bc", [128, D], F32)
    fing_bc = P.sbuf("fing_bc", [128, D], F32)
    G2_bc = P.sbuf("G2_bc", [128, D], F32)
    sh2_bc = P.sbuf("sh2_bc", [128, D], F32)
    c2t = P.sbuf("c2t", [128, 34], F32)
    thr_bc, pcol = c2t[:, 0:32], c2t[:, 32:33]
    h2tok = P.sbuf("h2tok", [128, NCH, D], BF16)
    comb_all = P.sbuf("comb_all", [128, NCH, 32], F32)
    pos_all = P.sbuf("pos_all", [128, NCH, 32], F32)
    w_all = P.sbuf("w_all", [128, NCH, 2], F32)
    idx_all = P.sbuf("idx_all", [128, NCH, 2], mybir.dt.int32)
    runc = P.sbuf("runc", [128, 32], F32)
    segs = P.sbuf("segs", [128, 32], F32)
    wk3 = P.sbuf("wk3", [128, 32, 32], F32)
    sm3 = P.sbuf("sm3", [128, 256], F32)
    IDXW = P.sbuf("IDXW", [128, 128], mybir.dt.int32)
    IDXA = P.sbuf("IDXA", [128, 128], mybir.dt.int32)
    IDXB = P.sbuf("IDXB", [128, 128], mybir.dt.int32)
    h2Tcs = [P.sbuf("h2Tc%d" % i, [128, 8, 128], BF16) for i in range(4)]
    rts = [P.sbuf("rts%d" % i, [128, 128], F32) for i in range(4)]
    ohts = [P.sbuf("oht%d" % i, [128, 32], F32) for i in range(4)]
    cnt_all = P.sbuf("cnt_all", [128, NCH, 32], F32)
    wr_bf = P.sbuf("wr_bf", [128, 8, 36], BF16)
    br_bc = P.sbuf("br_bc", [128, 36], F32)
    rt = P.sbuf("rt", [128, 128], F32)
    xstg = [P.sbuf("xstg%d" % i, [128, D], F32) for i in range(2)]
    ot = [P.sbuf("ot%d" % i, [128, D], F32) for i in range(2)]
    s3 = [P.sbuf("s3_%d" % i, [128, 4], F32) for i in range(4)]
    ph3a = contextlib.ExitStack()
    P.scope = ph3a
    wout3 = P.sbuf("wout3", [128, 16, D], BF16)
    ytc = [P.sbuf("ytc%d" % i, [128, 16, 128], BF16) for i in range(4)]
    xsa = [P.sbuf("xsa%d" % i, [128, D], F32) for i in range(4)]
    xna = [P.sbuf("xna%d" % i, [128, D], BF16) for i in range(4)]
    s3a = [P.sbuf("s3a%d" % i, [128, 4], F32) for i in range(4)]
    ngc3 = P.sbuf("ngc3", [128, 16], F32)
    gate1_bc = xsa[3]
    zt = P.sbuf("zt", [128, D], BF16)
    gb3 = ot[0]
    c3 = {"xs": 0}
    IOA = bass.IndirectOffsetOnAxis

    def bc_rows(colap, dst):
        op("dve", lambda e: e.tensor_copy(out=gb3[:].rearrange("p (j m) -> p j m", m=128), in_=bcl(colap, [128, 8, 128])),
           reads=[modc, G12], writes=[gb3])
        b_, pg3, pg3k = pbank(2)

        def mmg3(e):
            for j in range(8):
                ins = e.matmul(pg3[:, j * 128:(j + 1) * 128], lhsT=gb3[:, j * 128:(j + 1) * 128], rhs=ident_f, start=True, stop=True)
            return ins
        op("pe", mmg3, reads=[gb3, cf], writes=pg3k)
        op("act", lambda e: e.activation(out=dst[:], in_=pg3, func=AF.Copy), reads=pg3k, writes=[dst])
    bc_rows(modc[:, 40:48], gate2_bc)
    bc_rows(G12[:, 8:16], G2_bc)
    bc_rows(modc[:, 24:32], sh2_bc)
    bc_rows(modc[:, 16:24], gate1_bc)
    dma("sp", lambda e: e.dma_start(out=ngc3[:], in_=ngcol[:, :]), "c9", writes=[ngc3])
    for kc in range(16):
        wt = xstg[kc % 2]
        dma("sp", lambda e, wt=wt, kc=kc: e.dma_start(out=wt[:], in_=w_out[kc * 128:(kc + 1) * 128, :]), wt, writes=[wt])
        op("dve", lambda e, wt=wt, kc=kc: e.scalar_tensor_tensor(out=wout3[:, kc, :], in0=wt[:], scalar=ngc3[:, kc:kc + 1],
                                                                in1=gate1_bc[:], op0=ALU.mult, op1=ALU.mult),
           reads=[wt, ngc3, gate1_bc], writes=[(wout3, kc)])
    dma("sp", lambda e: e.dma_start(out=fing_bc[:], in_=fing.partition_broadcast(128)), "c3", writes=[fing_bc])
    dma("sp", lambda e: e.dma_start(out=c2t[:], in_=c2_d[:, :]), "c2", writes=[c2t])
    dma("pool", lambda e: e.dma_start(out=wr_bf[:], in_=w_r.rearrange("(k p) c -> p k c", p=128)), "wr", writes=[wr_bf])
    dma("sp", lambda e: e.dma_start(out=br_bc[:], in_=b_r.partition_broadcast(128)), "br", writes=[br_bc])
    op("dve", lambda e: e.memset(zt[:], 0.0), writes=[zt])
    hsz = hs_d.rearrange("(b p) d -> b p d", p=128)
    for b in range(NSLOT // 128):
        dma("act", lambda e, b=b: e.dma_start(out=hsz[b], in_=zt[:]), "hsz", reads=[zt])
    op("dve", lambda e: e.memset(runc[:], 0.0), writes=[runc])

    def xs_rot():
        i = c3["xs"]
        c3["xs"] += 1
        return xstg[i % 2]

    def router_chunk(c, sid):
        rt = rts[sid]
        h2Tc = h2Tcs[sid]
        prk = [("ps", 2 * sid + 1)]
        pr_ = PS[:, (2 * sid + 1) * 512:(2 * sid + 2) * 512]

        def mm(e):
            for k in range(8):
                ins = e.matmul(pr_[:, 0:36], lhsT=h2Tc[:, k, :], rhs=wr_bf[:, k, :], start=(k == 0), stop=(k == 7))
            return ins
        op("pe", mm, reads=[h2Tc, wr_bf], writes=prk)
        lg = rt[:, 0:36]
        R = [rt]
        op("dve", lambda e: e.tensor_tensor(out=lg, in0=pr_[:, 0:36], in1=br_bc[:], op=ALU.add), reads=prk + [br_bc], writes=R)
        gmax, gsum, pg = rt[:, 36:37], rt[:, 37:38], rt[:, 38:39]
        ohg = rt[:, 40:44]
        op("dve", lambda e: e.reduce_max(out=gmax, in_=rt[:, 0:4], axis=AX.X), reads=R, writes=R)
        op("dve", lambda e: e.tensor_scalar(out=ohg, in0=rt[:, 0:4], scalar1=gmax, scalar2=None, op0=ALU.is_equal), reads=R, writes=R)
        op("dve", lambda e: e.tensor_scalar(out=rt[:, 39:40], in0=gmax, scalar1=-1.0, scalar2=None, op0=ALU.mult), reads=R, writes=R)
        op("act", lambda e: e.activation(out=rt[:, 44:48], in_=rt[:, 0:4], func=AF.Exp, bias=rt[:, 39:40], scale=1.0, accum_out=gsum),
           reads=R, writes=R)
        op("dve", lambda e: e.reciprocal(out=pg, in_=gsum), reads=R, writes=R)
        msk = rt[:, 48:80]
        op("dve", lambda e: e.tensor_tensor(out=msk.rearrange("p (g e) -> p g e", e=8), in0=rt[:, 4:36].rearrange("p (g e) -> p g e", e=8),
                                            in1=bcl(ohg, [128, 4, 8]), op=ALU.mult), reads=R, writes=R)
        esel = rt[:, 80:88]
        op("dve", lambda e: e.tensor_reduce(out=esel, in_=msk.rearrange("p (g e) -> p e g", e=8), axis=AX.X, op=ALU.add), reads=R, writes=R)
        m8 = rt[:, 88:96]
        op("dve", lambda e: e.max(out=m8, in_=esel), reads=R, writes=R)
        op("dve", lambda e: e.tensor_scalar(out=rt[:, 96:97], in0=m8[:, 0:1], scalar1=-1.0, scalar2=None, op0=ALU.mult), reads=R, writes=R)
        tq = rt[:, 100:108]
        op("act", lambda e: e.activation(out=tq, in_=esel, func=AF.Exp, bias=rt[:, 96:97], scale=1.0), reads=R, writes=R)
        mk2 = rt[:, 108:116]
        op("dve", lambda e: e.tensor_scalar(out=mk2, in0=esel, scalar1=m8[:, 1:2], scalar2=None, op0=ALU.is_ge), reads=R, writes=R)
        op("dve", lambda e: e.tensor_tensor(out=tq, in0=tq, in1=mk2, op=ALU.mult), reads=R, writes=R)
        op("dve", lambda e: e.reduce_sum(out=rt[:, 97:98], in_=tq, axis=AX.X), reads=R, writes=R)
        op("dve", lambda e: e.reciprocal(out=rt[:, 98:99], in_=rt[:, 97:98]), reads=R, writes=R)
        op("dve", lambda e: e.tensor_tensor(out=rt[:, 98:99], in0=rt[:, 98:99], in1=pg, op=ALU.mult), reads=R, writes=R)
        op("dve", lambda e: e.tensor_scalar(out=tq, in0=tq, scalar1=rt[:, 98:99], scalar2=None, op0=ALU.mult), reads=R, writes=R)
        op("dve", lambda e: e.tensor_tensor(out=comb_all[:, c, :].rearrange("p (g e) -> p g e", e=8), in0=bcl(ohg, [128, 4, 8]),
                                            in1=bcm(tq, [128, 4, 8]), op=ALU.mult), reads=R, writes=[(comb_all, c)])

    def passA_chunk(c, sid):
        r0 = c * 128
        xs = xsa[sid]
        h2Tc = h2Tcs[sid]
        yt_ = ytc[sid]
        bO, bR = 2 * sid, 2 * sid + 1
        dma("sp", lambda e: e.dma_start(out=xs[:], in_=x[r0:r0 + 128, :]), xs, writes=[xs])
        dma("sp", lambda e: e.dma_start(out=yt_[:], in_=yT_d[c, :, :, :]), yt_, writes=[yt_])
        pok = [("ps", bO)]
        po_ = PS[:, bO * 512:(bO + 1) * 512]
        for n in range(2):
            def mmo(e, n=n):
                for kc in range(16):
                    ins = e.matmul(po_, lhsT=yt_[:, kc, :], rhs=wout3[:, kc, n * 512:(n + 1) * 512], start=(kc == 0), stop=(kc == 15))
                return ins
            op("pe", mmo, reads=[yt_] + [(wout3, kc) for kc in range(16)], writes=pok)
            op("dve", lambda e, n=n: e.tensor_tensor(out=xs[:, n * 512:(n + 1) * 512], in0=po_, in1=xs[:, n * 512:(n + 1) * 512], op=ALU.add),
               reads=pok + [xs], writes=[xs])
        dma("act", lambda e: e.dma_start(out=x1_d[r0:r0 + 128, :], in_=xs[:]), ("x1st", sid), reads=[xs])
        s4 = s3a[sid]
        op("act", lambda e: e.activation(out=junk[:], in_=xs[:], func=AF.Square, accum_out=s4[:, 0:1]), reads=[xs], writes=[s4, "junk"])
        rstd_ops(s4[:, 0:1], s4[:, 1:2], D, [s4])
        xn = xna[sid]
        op("dve", lambda e: e.scalar_tensor_tensor(out=xn[:], in0=xs[:], scalar=s4[:, 1:2], in1=G2_bc[:],
                                                   op0=ALU.mult, op1=ALU.mult), reads=[xs, s4, G2_bc], writes=[xn])
        op("dve", lambda e: e.tensor_tensor(out=h2tok[:, c, :], in0=xn[:], in1=sh2_bc[:], op=ALU.add),
           reads=[xn, sh2_bc], writes=[(h2tok, c)])
        ptk = [("ps", bR)]
        ptb = PS[:, bR * 512:(bR + 1) * 512].bitcast(BF16)

        def tr(e):
            for k in range(8):
                ins = e.transpose(out=ptb[:, k * 128:(k + 1) * 128], in_=h2tok[:, c, k * 128:(k + 1) * 128], identity=ident_bf)
            return ins
        op("pe", tr, reads=[(h2tok, c), cbf], writes=ptk)
        op("act", lambda e: e.activation(out=h2Tc[:].rearrange("p k t -> p (k t)"), in_=ptb, func=AF.Copy), reads=ptk, writes=[h2Tc])
        router_chunk(c, sid)
        oht = ohts[sid]
        op("dve", lambda e: e.tensor_scalar(out=oht[:], in0=comb_all[:, c, :], scalar1=0.0, scalar2=None, op0=ALU.is_gt),
           reads=[(comb_all, c)], writes=[oht])
        pk4 = [("ps", bR)]
        p4 = PS[:, bR * 512:(bR + 1) * 512]
        op("pe", lambda e: e.matmul(p4[:, 64:96], lhsT=SL_f, rhs=oht[:], start=True, stop=True), reads=[oht, cf], writes=pk4)
        op("pe", lambda e: e.matmul(p4[:, 96:128], lhsT=ones_f, rhs=oht[:], start=True, stop=True), reads=[oht, cf] + pk4, writes=pk4)
        op("dve", lambda e: e.tensor_copy(out=pos_all[:, c, :], in_=p4[:, 64:96]), reads=pk4, writes=[(pos_all, c)])
        op("dve", lambda e: e.tensor_copy(out=cnt_all[:, c, :], in_=p4[:, 96:128]), reads=pk4, writes=[(cnt_all, c)])
    for c in range(0, NCH, 4):
        P.interleave([lambda c=c, i=i: passA_chunk(c + i, i) for i in range(4)], [3, 3, 3, 3])
    for c in range(NCH):
        op("dve", lambda e, c=c: e.tensor_tensor(out=pos_all[:, c, :], in0=pos_all[:, c, :], in1=runc[:], op=ALU.add),
           reads=[(pos_all, c), runc], writes=[(pos_all, c)])
        op("dve", lambda e, c=c: e.tensor_tensor(out=runc[:], in0=cnt_all[:, c, :], in1=runc[:], op=ALU.add),
           reads=[(cnt_all, c), runc], writes=[runc])
    op("dve", lambda e: e.tensor_tensor(out=wk3[:], in0=bcl(runc[:], [128, 32, 32]), in1=bcm(thr_bc, [128, 32, 32]), op=ALU.is_gt),
       reads=[runc, c2t], writes=[wk3])
    nt = sm3[:, 32:64]
    op("dve", lambda e: e.tensor_reduce(out=nt, in_=wk3[:], axis=AX.X, op=ALU.add), reads=[wk3], writes=[sm3])
    Dm = sm3[0:32, 64:96]
    op("dve", lambda e: e.tensor_tensor(out=Dm, in0=ident_f[0:32, 0:32], in1=sm3[0:32, 32:64], op=ALU.mult), reads=[sm3, cf], writes=[sm3])
    colv = sm3[0:32, 96:97]
    op("dve", lambda e: e.reduce_sum(out=colv, in_=Dm, axis=AX.X), reads=[sm3], writes=[sm3])
    lbt = wk3[0:32, 0, :].rearrange("p (a b) -> p a b", b=32)
    lb = wk3[0:32, 0:4, :].rearrange("p a b -> p (a b)")
    op("dve", lambda e: e.tensor_copy(out=lb, in_=colv.to_broadcast([32, 128])), reads=[sm3, wk3], writes=[wk3])
    pk4 = [("ps", 4)]
    p4 = PS[:, 4 * 512:5 * 512]
    op("pe", lambda e: e.matmul(p4[:, 64:96], lhsT=lb, rhs=SL_f[0:32, 0:32], start=True, stop=True), reads=[wk3, cf], writes=pk4)
    op("dve", lambda e: e.tensor_scalar(out=segs[:], in0=p4[:, 64:96], scalar1=float(TSL), scalar2=None, op0=ALU.mult), reads=pk4, writes=[segs])
    cumi = sm3[:, 128:160]
    op("dve", lambda e: e.tensor_tensor(out=cumi, in0=p4[:, 64:96], in1=nt, op=ALU.add), reads=pk4 + [sm3], writes=[sm3])
    cmpj = sm3[:, 160:192]
    op("dve", lambda e: e.tensor_scalar(out=cmpj, in0=cumi, scalar1=pcol, scalar2=None, op0=ALU.is_le), reads=[sm3, c2t], writes=[sm3])
    ej = sm3[:, 192:193]
    op("dve", lambda e: e.reduce_sum(out=ej, in_=cmpj, axis=AX.X), reads=[sm3], writes=[sm3])
    op("dve", lambda e: e.tensor_scalar(out=ej, in0=ej, scalar1=31.0, scalar2=None, op0=ALU.min), reads=[sm3], writes=[sm3])
    dgj = wk3[:, 1, :]
    dgj = wk3[:, 4:8, :].rearrange("p a b -> p (a b)")
    op("dve", lambda e: e.tensor_scalar(out=dgj, in0=ident_f, scalar1=ej, scalar2=None, op0=ALU.mult), reads=[sm3, cf, wk3], writes=[wk3])
    op("pe", lambda e: e.matmul(p4[:, 128:256], lhsT=ones_f, rhs=dgj, start=True, stop=True), reads=[wk3, cf] + pk4, writes=pk4)
    idxf = wk3[:, 8:12, :].rearrange("p a b -> p (a b)")
    op("dve", lambda e: e.tensor_scalar(out=idxf, in0=p4[:, 128:256], scalar1=128.0, scalar2=pcol, op0=ALU.mult, op1=ALU.add),
       reads=pk4 + [c2t, wk3], writes=[wk3])
    op("dve", lambda e: e.tensor_copy(out=IDXW[:], in_=idxf), reads=[wk3], writes=[IDXW])
    op("dve", lambda e: e.tensor_scalar(out=idxf, in0=idxf, scalar1=2.0, scalar2=None, op0=ALU.mult), reads=[wk3, IDXW], writes=[wk3])
    op("dve", lambda e: e.tensor_copy(out=IDXA[:], in_=idxf), reads=[wk3], writes=[IDXA])
    op("dve", lambda e: e.tensor_scalar(out=idxf, in0=idxf, scalar1=1.0, scalar2=None, op0=ALU.add), reads=[wk3, IDXA], writes=[wk3])
    op("dve", lambda e: e.tensor_copy(out=IDXB[:], in_=idxf), reads=[wk3], writes=[IDXB])
    P.flush()
    for c in range(NCH):
        R = [rt]
        sf, oht, ms, m8 = rt[:, 0:32], rt[:, 32:64], rt[:, 64:96], rt[:, 96:104]
        op("dve", lambda e, c=c: e.tensor_tensor(out=sf, in0=pos_all[:, c, :], in1=segs[:], op=ALU.add), reads=[(pos_all, c), segs] + R, writes=R)
        op("dve", lambda e, c=c: e.tensor_scalar(out=oht, in0=comb_all[:, c, :], scalar1=0.0, scalar2=None, op0=ALU.is_gt),
           reads=[(comb_all, c)] + R, writes=R)
        op("dve", lambda e: e.scalar_tensor_tensor(out=ms, in0=sf, scalar=1.0, in1=oht, op0=ALU.add, op1=ALU.mult), reads=R, writes=R)
        op("dve", lambda e: e.max(out=m8, in_=ms), reads=R, writes=R)
        for k in range(2):
            eq = rt[:, 104:136] if False else sm3[:, 200:232]
            op("dve", lambda e, k=k: e.tensor_scalar(out=eq, in0=ms, scalar1=m8[:, k:k + 1], scalar2=None, op0=ALU.is_equal),
               reads=R + [sm3], writes=[sm3])
            op("dve", lambda e, c=c: e.tensor_tensor(out=eq, in0=eq, in1=comb_all[:, c, :], op=ALU.mult), reads=[sm3, (comb_all, c)], writes=[sm3])
            op("dve", lambda e, c=c, k=k: e.reduce_sum(out=w_all[:, c, k:k + 1], in_=eq, axis=AX.X), reads=[sm3], writes=[(w_all, c)])
        op("dve", lambda e: e.tensor_scalar(out=rt[:, 104:106], in0=m8[:, 0:2], scalar1=-1.0, scalar2=None, op0=ALU.add), reads=R, writes=R)
        op("dve", lambda e, c=c: e.tensor_copy(out=idx_all[:, c, :], in_=rt[:, 104:106]), reads=R, writes=[(idx_all, c)])
        for k in range(2):
            if stop == "idx":
                continue
            dma("pool", lambda e, c=c, k=k: e.indirect_dma_start(out=hs_d[:, :], out_offset=IOA(ap=idx_all[:, c, k:k + 1], axis=0),
                                                                in_=h2tok[:, c, :], in_offset=None),
                "hsc", reads=[(idx_all, c), (h2tok, c)])
    if stop == "idx":
        dump("idx_all", idx_all[:], [(idx_all, c) for c in range(NCH)])
        dump("w_all", w_all[:], [(w_all, c) for c in range(NCH)])
        dump("IDXW", IDXW[:], [IDXW])
        dump("segs", segs[:], [segs])
        dump("runc", runc[:], [runc])
        dump("comb_all", comb_all[:], [(comb_all, c) for c in range(NCH)])
        dump("pos_all", pos_all[:], [(pos_all, c) for c in range(NCH)])
        P.flush()
        ph3a.close()
        ph3.close()
        P.base.close()
        return nc
    P.flush()
    if stop == "scatter":
        ph3a.close()
        ph3.close()
        P.base.close()
        return nc
    ph3a.close()
    ph3t = contextlib.ExitStack()
    P.scope = ph3t
    w13t = [P.sbuf("w13t%d" % i, [128, 8, 512], BF16) for i in range(3)]
    w2t = [P.sbuf("w2t%d" % i, [128, 2, D], BF16) for i in range(3)]
    hst = [P.sbuf("hst%d" % i, [128, D], BF16) for i in range(4)]
    hsT = [P.sbuf("hsT%d" % i, [128, 8, 128], BF16) for i in range(4)]
    s1t = [P.sbuf("s1t%d" % i, [128, 256], BF16) for i in range(4)]
    at = [P.sbuf("at%d" % i, [128, 256], BF16) for i in range(4)]
    aTt = [P.sbuf("aTt%d" % i, [128, 256], BF16) for i in range(4)]
    yst = [P.sbuf("yst%d" % i, [128, D], BF16) for i in range(2)]
    w13v = w13c_d
    w2v = w2c_d

    def load_w(j):
        b = j % 3
        for hf, IX in ((0, IDXA), (1, IDXB)):
            dma("pool", lambda e, hf=hf, IX=IX: e.indirect_dma_start(
                out=w13t[b][:, 4 * hf:4 * hf + 4, :].rearrange("p k c -> p (k c)"), out_offset=None, in_=w13v[:, :],
                in_offset=IOA(ap=IX[:, j:j + 1], axis=0)), ("w13", b), reads=[IX], writes=[w13t[b]])
        dma("pool", lambda e: e.indirect_dma_start(out=w2t[b][:].rearrange("p h c -> p (h c)"), out_offset=None, in_=w2v[:, :],
                                                   in_offset=IOA(ap=IDXW[:, j:j + 1], axis=0)), ("w2", b), reads=[IDXW], writes=[w2t[b]])
    NI = NTL * (TSL // 128)
    SPT = TSL // 128

    def st_L(i):
        r0 = i * 128
        hs_ = hst[i % 4]
        dma("sp", lambda e: e.dma_start(out=hs_[:], in_=hs_d[r0:r0 + 128, :]), hs_, writes=[hs_])

    def st_T(i):
        q4 = i % 4
        hs_ = hst[q4]
        bT = 6 + i % 2
        ptk = [("ps", bT)]
        ptb = PS[:, bT * 512:(bT + 1) * 512].bitcast(BF16)

        def tr(e):
            for k in range(8):
                ins = e.transpose(out=ptb[:, k * 128:(k + 1) * 128], in_=hs_[:, k * 128:(k + 1) * 128], identity=ident_bf)
            return ins
        op("pe", tr, reads=[hs_, cbf], writes=ptk)
        hT_ = hsT[q4]
        op("act", lambda e: e.activation(out=hT_[:].rearrange("p k t -> p (k t)"), in_=ptb, func=AF.Copy), reads=ptk, writes=[hT_])

    def st_H(i):
        q4 = i % 4
        b = (i // SPT) % 3
        hT_ = hsT[q4]
        bA = 4 + i % 2
        ph_, phk = PS[:, bA * 512:(bA + 1) * 512], [("ps", bA)]

        def mmh(e):
            for k in range(8):
                ins = e.matmul(ph_, lhsT=hT_[:, k, :], rhs=w13t[b][:, k, :], start=(k == 0), stop=(k == 7))
            return ins
        op("pe", mmh, reads=[hT_, w13t[b]], writes=phk)
        s1, a_ = s1t[q4], at[q4]
        op("act", lambda e: e.activation(out=s1[:], in_=ph_[:, 0:256], func=AF.Silu), reads=phk, writes=[s1])
        op("dve", lambda e: e.tensor_tensor(out=a_[:], in0=ph_[:, 256:512], in1=s1[:], op=ALU.mult), reads=phk + [s1], writes=[a_])

    def st_B(i):
        q4 = i % 4
        a_, aT_ = at[q4], aTt[q4]
        bA = 4 + i % 2
        pt7k = [("ps", bA)]
        pt7b = PS[:, bA * 512:bA * 512 + 128].bitcast(BF16)

        def tr2(e):
            for hh in range(2):
                ins = e.transpose(out=pt7b[:, hh * 128:(hh + 1) * 128], in_=a_[:, hh * 128:(hh + 1) * 128], identity=ident_bf)
            return ins
        op("pe", tr2, reads=[a_, cbf], writes=pt7k)
        op("act", lambda e: e.activation(out=aT_[:], in_=pt7b[:, 0:256], func=AF.Copy), reads=pt7k, writes=[aT_])

    def st_Y(i):
        q4 = i % 4
        q = i % 2
        b = (i // SPT) % 3
        r0 = i * 128
        aT_ = aTt[q4]
        pa, pak = PS[:, q * 1024:(q + 1) * 1024], [("ps", 2 * q), ("ps", 2 * q + 1)]

        def mmy(e):
            for n in range(2):
                for hh in range(2):
                    ins = e.matmul(pa[:, n * 512:(n + 1) * 512], lhsT=aT_[:, hh * 128:(hh + 1) * 128],
                                   rhs=w2t[b][:, hh, n * 512:(n + 1) * 512], start=(hh == 0), stop=(hh == 1))
            return ins
        op("pe", mmy, reads=[aT_, w2t[b]], writes=pak)
        ys_ = yst[q]
        op("act", lambda e: e.activation(out=ys_[:], in_=pa, func=AF.Copy), reads=pak, writes=[ys_])
        dma("pool", lambda e: e.dma_start(out=ys_d[r0:r0 + 128, :], in_=ys_[:]), ("yst", q), reads=[ys_])

    load_w(0)
    load_w(1)
    st_L(0)
    st_L(1)
    for n in range(NI + 3):
        if n + 2 < NI:
            st_L(n + 2)
        if n < NI:
            st_T(n)
        if 0 <= n - 1 < NI:
            st_H(n - 1)
        if 0 <= n - 2 < NI:
            st_B(n - 2)
        if 0 <= n - 3 < NI:
            st_Y(n - 3)
        if n % SPT == 0 and n >= SPT:
            jn = n // SPT + 1
            if jn < NTL:
                load_w(jn)
    P.flush()
    if stop == "tiles":
        ph3t.close()
        ph3.close()
        P.base.close()
        return nc
    ph3t.close()
    ph3c = contextlib.ExitStack()
    P.scope = ph3c
    yg = [P.sbuf("yg%d" % i, [128, D], BF16) for i in range(8)]
    otc = [P.sbuf("otc%d" % i, [128, D], F32) for i in range(4)]
    xsc = [P.sbuf("xsc%d" % i, [128, D], F32) for i in range(4)]
    s3c = [P.sbuf("s3c%d" % i, [128, 4], F32) for i in range(4)]
    def combine_chunk(c, sid):
        r0 = c * 128
        o_ = otc[sid]
        s4 = s3c[sid]
        xs = xsc[sid]
        dma("sp", lambda e: e.dma_start(out=xs[:], in_=x1_d[r0:r0 + 128, :]), xs, writes=[xs])
        ygs = [yg[2 * sid + k] for k in range(2)]
        for k in range(2):
            dma("pool", lambda e, k=k: e.indirect_dma_start(out=ygs[k][:], out_offset=None, in_=ys_d[:, :],
                                                            in_offset=IOA(ap=idx_all[:, c, k:k + 1], axis=0)),
                ygs[k], reads=[(idx_all, c)], writes=[ygs[k]])
        op("act", lambda e: e.activation(out=o_[:], in_=ygs[0][:], func=AF.Copy, scale=w_all[:, c, 0:1]),
           reads=[ygs[0], (w_all, c)], writes=[o_])
        op("dve", lambda e: e.scalar_tensor_tensor(out=o_[:], in0=ygs[1][:], scalar=w_all[:, c, 1:2], in1=o_[:],
                                                   op0=ALU.mult, op1=ALU.add), reads=[ygs[1], (w_all, c), o_], writes=[o_])
        op("dve", lambda e: e.tensor_tensor(out=o_[:], in0=o_[:], in1=gate2_bc[:], op=ALU.mult), reads=[o_, gate2_bc], writes=[o_])
        op("dve", lambda e: e.tensor_tensor(out=o_[:], in0=o_[:], in1=xs[:], op=ALU.add), reads=[o_, xs], writes=[o_])
        op("act", lambda e: e.activation(out=junk[:], in_=o_[:], func=AF.Square, accum_out=s4[:, 2:3]), reads=[o_, s4], writes=[s4, "junk"])
        rstd_ops(s4[:, 2:3], s4[:, 3:4], D, [s4])
        op("dve", lambda e: e.scalar_tensor_tensor(out=o_[:], in0=o_[:], scalar=s4[:, 3:4], in1=fing_bc[:],
                                                   op0=ALU.mult, op1=ALU.mult), reads=[o_, s4, fing_bc], writes=[o_])
        dma("act", lambda e: e.dma_start(out=out[r0:r0 + 128, :], in_=o_[:]), ("ost", sid), reads=[o_])
    for c in range(0, NCH, 4):
        P.interleave([lambda c=c, i=i: combine_chunk(c + i, i) for i in range(4)], [3, 3, 3, 3])
    P.flush()
    ph3c.close()
    ph3.close()
    P.base.close()
    return nc


def _consts():
    i = np.arange(128)
    ident = np.eye(128, dtype=np.float32)
    ones = np.ones((128, 128), np.float32)
    U = (i[:, None] <= i[None, :]).astype(np.float32)
    L = (i[:, None] >= i[None, :]).astype(np.float32)
    SL = (i[:, None] < i[None, :]).astype(np.float32)
    mF = np.where(i[:, None] <= i[None, :], 0.0, NEG).astype(np.float32)
    mB = np.where(i[:, None] >= i[None, :], 0.0, NEG).astype(np.float32)
    cbf = np.stack([ident, ones, U, L, mF, mB], axis=1).astype(ml_dtypes.bfloat16)
    cf = np.stack([ident, ones, U, L, SL], axis=1).astype(np.float32)
    return np.ascontiguousarray(cbf), np.ascontiguousarray(cf)


def _cols(v, n):
    return np.ascontiguousarray(np.asarray(v, np.float32).reshape(n, 128).T)


def prep_inputs(inp, b):
    f = lambda a: np.ascontiguousarray(np.asarray(a, np.float32))
    cbf, cf = _consts()
    m = {}
    m["x"] = f(inp["x"][b])
    m["ccol"] = _cols(inp["c"][b], 8)
    m["w_ada"] = f(inp["w_ada"][0])
    m["badac"] = _cols(inp["b_ada"][0], 48)
    m["n12g"] = np.ascontiguousarray(np.concatenate([_cols(inp["norm1_g"][0], 8), _cols(inp["norm2_g"][0], 8)], axis=1))
    m["w_in"] = f(inp["w_in"][0])
    m["b_in"] = f(inp["b_in"][0]).reshape(1, -1)
    m["bxbc"] = _cols(np.asarray(inp["b_in"][0])[3072:4608], 12)
    m["lngb"] = np.ascontiguousarray(np.stack([f(inp["gmlp_ln_g"][0]), f(inp["gmlp_ln_b"][0])], axis=0))
    m["w_sT"] = np.ascontiguousarray(np.transpose(f(inp["gmlp_w_s"][0]), (2, 0, 1)))
    m["b_s2"] = np.ascontiguousarray(f(inp["gmlp_b_s"][0]).T)
    m["ngcol"] = np.ascontiguousarray(np.concatenate([_cols(inp["gmlp_out_g"][0], 8), _cols(inp["ssd_norm_g"][0], 8)], axis=1))
    cw = f(inp["conv_w"][0])
    m["convw"] = np.ascontiguousarray(np.transpose(cw.reshape(5, 12, 128), (2, 1, 0)))
    m["convb"] = _cols(inp["conv_b"][0], 12)
    m["ssdv"] = np.ascontiguousarray(np.concatenate([f(inp["a_log_f"][0]), f(inp["a_log_b"][0]), f(inp["dt_bias_f"][0]),
                                                     f(inp["dt_bias_b"][0])]).reshape(1, 64))
    m["dskc"] = _cols(np.repeat(f(inp["d_skip"][0]), 64), 8)
    m["w_out"] = f(inp["w_out"][0])
    wre = f(inp["w_router_e"][0])
    m["w_r"] = np.ascontiguousarray(np.concatenate([f(inp["w_router_g"][0])] + [wre[g] for g in range(4)], axis=1))
    m["b_r"] = np.ascontiguousarray(np.concatenate([f(inp["b_router_g"][0]), f(inp["b_router_e"][0]).reshape(-1)]).reshape(1, 36))
    w13 = np.concatenate([f(inp["w1"][0]), f(inp["w3"][0])], axis=2)
    m["w13p"] = np.ascontiguousarray(np.transpose(w13.reshape(32, 8, 128, 512), (0, 2, 1, 3)).reshape(8192, 2048))
    m["w2p"] = np.ascontiguousarray(np.transpose(f(inp["w2"][0]).reshape(32, 2, 128, D), (0, 2, 1, 3)).reshape(4096, 2048))
    c2 = np.zeros((128, 34), np.float32)
    c2[:, 0:32] = 256.0 * np.arange(32, dtype=np.float32)[None, :]
    c2[:, 32] = np.arange(128, dtype=np.float32)
    m["c2"] = c2
    m["fing"] = f(inp["final_g"]).reshape(1, -1)
    m["cbf"] = cbf
    m["cf"] = cf
    return m


_NC_CACHE = {}


def kernel(**inputs):
    x = np.asarray(inputs["x"])
    B, S, _ = x.shape
    if S not in _NC_CACHE:
        _NC_CACHE[S] = build_nc(S)
    nc = _NC_CACHE[S]
    shared = None
    in_maps = []
    for b in range(B):
        if shared is None:
            m = prep_inputs(inputs, b)
            shared = m
        else:
            m = dict(shared)
            m["x"] = np.ascontiguousarray(x[b], dtype=np.float32)
            m["ccol"] = _cols(np.asarray(inputs["c"])[b], 8)
        in_maps.append(m)
    res = run_bass_kernel_spmd(nc, in_maps, core_ids=list(range(B)))
    return np.stack([np.asarray(r["out"], dtype=np.float32) for r in res.results], axis=0)
```

```python
import contextlib
import threading
import numpy as np
import ml_dtypes
import concourse.bass as bass
import concourse.mybir as mybir
from concourse.bass_utils import run_bass_kernel_spmd

F32 = mybir.dt.float32
BF16 = mybir.dt.bfloat16
ALU = mybir.AluOpType
AF = mybir.ActivationFunctionType
AX = mybir.AxisListType

D = 1024
NCORES = 8
EPS = 1e-6
NEG = -30000.0
ENGS = ("pe", "act", "dve", "pool", "sp")
HND = {"pe": "tensor", "act": "scalar", "dve": "vector", "pool": "gpsimd", "sp": "sync"}


class Op:
    __slots__ = ("eng", "fn", "deps", "sig", "is_dma", "dsem", "dval", "grp")


class Prog:
    def __init__(self, nc, same_engine_sync=True):
        self.nc = nc
        self.ops = {e: [] for e in ENGS}
        self.last_w = {}
        self.readers = {}
        self.ses = same_engine_sync
        self.dma_sems = {}
        self.base = contextlib.ExitStack()
        self.esem = {e: self.base.enter_context(nc.semaphore("e_" + e)) for e in ENGS}
        self.cnt = {e: 0 for e in ENGS}
        self.waited = {e: {} for e in ENGS}
        self.scope = self.base
        self._il = None
        self._tl = threading.local()

    def stream(self):
        return getattr(self._tl, "idx", None) if self._il is not None else None

    def _next_turn(self, i):
        st = self._il["st"]
        n = len(st["done"])
        for d in range(1, n + 1):
            j = (i + d) % n
            if not st["done"][j]:
                st["turn"] = j
                return
        st["turn"] = -1

    def interleave(self, fns, quanta):
        n = len(fns)
        st = {"turn": 0, "done": [False] * n, "err": None}
        cv = threading.Condition()
        self._il = {"st": st, "cv": cv, "quanta": quanta, "used": [0] * n}

        def runner(i):
            self._tl.idx = i
            with cv:
                while st["turn"] != i:
                    cv.wait()
            try:
                fns[i]()
            except BaseException as ex:
                st["err"] = ex
            with cv:
                st["done"][i] = True
                self._next_turn(i)
                cv.notify_all()
        ths = [threading.Thread(target=runner, args=(i,)) for i in range(n)]
        for t in ths:
            t.start()
        for t in ths:
            t.join()
        self._il = None
        if st["err"] is not None:
            raise st["err"]

    def _yield(self):
        il = self._il
        if il is None:
            return
        i = getattr(self._tl, "idx", None)
        if i is None:
            return
        il["used"][i] += 1
        if il["used"][i] > il["quanta"][i]:
            il["used"][i] = 1
            st, cv = il["st"], il["cv"]
            with cv:
                self._next_turn(i)
                cv.notify_all()
                while st["turn"] != i:
                    cv.wait()

    def sbuf(self, name, shape, dtype):
        return self.scope.enter_context(self.nc.sbuf_tensor(name, list(shape), dtype))

    def psum(self, name, shape, dtype):
        return self.scope.enter_context(self.nc.psum_tensor(name, list(shape), dtype))

    @staticmethod
    def _k(k):
        if isinstance(k, tuple):
            return tuple(Prog._k(x) for x in k)
        if isinstance(k, (str, int)):
            return k
        return k.name

    def op(self, eng, fn, reads=(), writes=(), _noyield=False):
        if not _noyield:
            self._yield()
        reads = [self._k(k) for k in reads]
        writes = [self._k(k) for k in writes]
        o = Op()
        o.eng, o.fn, o.is_dma, o.sig, o.dsem, o.dval = eng, fn, False, None, None, 0
        deps = []
        for k in reads:
            w = self.last_w.get(k)
            if w is not None:
                deps.append(w)
        for k in writes:
            w = self.last_w.get(k)
            if w is not None:
                deps.append(w)
            deps.extend(self.readers.get(k, ()))
        o.deps = [(d.dsem, self.dma_sems[d.grp][1]) if d.is_dma else d for d in deps]
        self.ops[eng].append(o)
        for k in reads:
            self.readers.setdefault(k, []).append(o)
        for k in writes:
            self.last_w[k] = o
            self.readers[k] = []
        return o

    def dma(self, eng, fn, group, reads=(), writes=()):
        self._yield()
        o = self.op(eng, fn, reads, writes, _noyield=True)
        o.is_dma = True
        group = self._k(group)
        if group not in self.dma_sems:
            self.dma_sems[group] = [self.base.enter_context(self.nc.semaphore("d%d" % len(self.dma_sems))), 0]
        ent = self.dma_sems[group]
        ent[1] += 16
        o.dsem, o.dval, o.grp = ent[0], ent[1], group
        return o

    def _skip_same(self, d, e):
        return d.eng == e and (not self.ses or e in ("pe", "sp"))

    def flush(self):
        for e in ENGS:
            for o in self.ops[e]:
                for d in o.deps:
                    if isinstance(d, tuple) or self._skip_same(d, o.eng):
                        continue
                    if d.sig is None:
                        d.sig = 0
            for o in reversed(self.ops[e]):
                if not o.is_dma:
                    if o.sig is None:
                        o.sig = 0
                    break
        for e in ENGS:
            for o in self.ops[e]:
                if o.sig == 0:
                    self.cnt[e] += 1
                    o.sig = self.cnt[e]
        with self.nc.Block() as block:
            for e in ENGS:
                def body(h, e=e, ops=self.ops[e]):
                    waited = self.waited[e]
                    for o in ops:
                        need = {}
                        for d in o.deps:
                            if isinstance(d, tuple):
                                sem, val = d
                                key = ("d", sem.name if hasattr(sem, "name") else id(sem))
                            else:
                                if not d.sig or self._skip_same(d, e):
                                    continue
                                key, sem, val = ("e", d.eng), self.esem[d.eng], d.sig
                            if waited.get(key, 0) >= val:
                                continue
                            if key not in need or need[key][1] < val:
                                need[key] = (sem, val)
                        for key, (sem, val) in need.items():
                            h.wait_ge(sem, val)
                            waited[key] = val
                        ins = o.fn(h)
                        if o.is_dma:
                            ins.then_inc(o.dsem, 16)
                        elif o.sig:
                            ins.then_inc(self.esem[e], 1)
                    for e2 in ENGS:
                        if e2 != e and self.cnt[e2] > waited.get(("e", e2), 0):
                            h.wait_ge(self.esem[e2], self.cnt[e2])
                            waited[("e", e2)] = self.cnt[e2]
                    for g, (sem, tot) in self.dma_sems.items():
                        key = ("d", sem.name if hasattr(sem, "name") else id(sem))
                        if tot > waited.get(key, 0):
                            h.wait_ge(sem, tot)
                            waited[key] = tot
                getattr(block, HND[e])(body)
        self.ops = {e: [] for e in ENGS}
        self.last_w = {}
        self.readers = {}


def bcl(ap, shape):
    return ap.unsqueeze(2).to_broadcast(list(shape))


def bcm(ap, shape):
    return ap.unsqueeze(1).to_broadcast(list(shape))


def build_nc(S, debug=None, stop=None):
    TL = 256
    CPT = TL // 128
    NT = S // TL
    NCH = S // 128
    TS = min(S, 1024)
    NSUP = S // TS
    CHS = TS // 128
    nc = bass.Bass("TRN2", target_bir_lowering=False)

    def din(name, shape, dt=F32):
        return nc.dram_tensor(name, list(shape), dt, kind="ExternalInput").ap()

    x = din("x", [S, D])
    ccol = din("ccol", [128, 8])
    w_ada = din("w_ada", [D, 6 * D])
    badac = din("badac", [128, 48])
    n12g = din("n12g", [128, 16])
    w_in = din("w_in", [D, 4640])
    b_in = din("b_in", [1, 4640])
    bxbc = din("bxbc", [128, 12])
    lngb = din("lngb", [2, D])
    w_sT = din("w_sT", [128, 8, 128])
    b_s2 = din("b_s2", [128, 8])
    ngcol = din("ngcol", [128, 16])
    convw = din("convw", [128, 12, 5])
    convb = din("convb", [128, 12])
    ssdv = din("ssdv", [1, 64])
    dskc = din("dskc", [128, 8])
    w_out = din("w_out", [2 * D, D])
    w_r = din("w_r", [D, 36])
    b_r = din("b_r", [1, 36])
    w13p = din("w13p", [32 * 128 * 2, 2048])
    w2p = din("w2p", [32 * 128, 2048])
    c2_d = din("c2", [128, 34])
    fing = din("fing", [1, D])
    cbf_d = din("cbf", [128, 6, 128], BF16)
    cf_d = din("cf", [128, 5, 128])
    out = nc.dram_tensor("out", [S, D], F32, kind="ExternalOutput").ap()
    skind = "ExternalOutput" if debug else "Internal"
    prevb_d = nc.dram_tensor("prevb_s", [NCH, 128, D], BF16, kind=skind).ap()
    x1_d = nc.dram_tensor("x1_s", [S, D], F32, kind=skind).ap()
    yT_d = nc.dram_tensor("yT_s", [NCH, 128, 16, 128], BF16, kind="Internal").ap()
    w13c_d = nc.dram_tensor("w13c_s", [32 * 128 * 2, 2048], BF16, kind="Internal").ap()
    w2c_d = nc.dram_tensor("w2c_s", [32 * 128, 2048], BF16, kind="Internal").ap()

    def dump(name, tap, keys):
        if not debug:
            return
        t = nc.dram_tensor("dbg_" + name, list(tap.shape), tap.dtype, kind="ExternalOutput").ap()
        P.dma("sp", lambda e: e.dma_start(out=t, in_=tap), "dbg_" + name, reads=keys)

    P = Prog(nc)
    op, dma = P.op, P.dma

    cbf = P.sbuf("cbf_t", [128, 6, 128], BF16)
    cf = P.sbuf("cf_t", [128, 5, 128], F32)
    ident_bf, ones_bf, U_bf, L_bf, mF_bf, mB_bf = [cbf[:, i, :] for i in range(6)]
    ident_f, ones_f, U_f, L_f, SL_f = [cf[:, i, :] for i in range(5)]
    modc = P.sbuf("modc", [128, 48], F32)
    G12 = P.sbuf("G12", [128, 16], F32)
    n12 = P.sbuf("n12", [128, 16], F32)
    junkb = P.sbuf("junkb", [128, D], BF16)
    junk = junkb
    PS = P.psum("PS", [128, 8 * 512], F32)
    psn = [0, 0, 0, 0]

    def pbank(n=1):
        sid = P.stream()
        if sid is None:
            b = psn[0]
            if b + n > 8:
                b = 0
            psn[0] = (b + n) % 8
        else:
            lo, nb = ((0, 4), (4, 2), (6, 2))[sid]
            b = psn[1 + sid]
            if b + n > nb:
                b = 0
            psn[1 + sid] = (b + n) % nb
            b += lo
        return b, PS[:, b * 512:(b + n) * 512], [("ps", b + i) for i in range(n)]

    dma("sp", lambda e: e.dma_start(out=cbf[:], in_=cbf_d[:, :, :]), "c0", writes=[cbf])
    dma("sp", lambda e: e.dma_start(out=cf[:], in_=cf_d[:, :, :]), "c1", writes=[cf])
    dma("sp", lambda e: e.dma_start(out=n12[:], in_=n12g[:, :]), "c2", writes=[n12])

    ph12 = contextlib.ExitStack()
    P.scope = ph12
    win_bf = P.sbuf("win_bf", [128, 8, 4640], BF16)
    bias3 = P.sbuf("bias3", [65, D], BF16)
    bxbc_t = P.sbuf("bxbc_t", [128, 12], F32)
    convw_t = P.sbuf("convw_t", [128, 12, 5], F32)
    convb_t = P.sbuf("convb_t", [128, 12], F32)
    dtb_bc = P.sbuf("dtb_bc", [128, 32], F32)
    a_bc = P.sbuf("a_bc", [128, 32], F32)

    ph0 = contextlib.ExitStack()
    P.scope = ph0
    cc_t = P.sbuf("cc_t", [128, 8], F32)
    cact = P.sbuf("cact", [128, 8], F32)
    badt = P.sbuf("badt", [128, 48], F32)
    wada = [P.sbuf("wada%d" % i, [128, 8, 512], F32) for i in range(2)]
    gb = P.sbuf("gb", [128, 8, 128], F32)
    tmp32 = P.sbuf("tmp32", [128, 64], F32)
    dma("sp", lambda e: e.dma_start(out=cc_t[:], in_=ccol[:, :]), "c4", writes=[cc_t])
    dma("sp", lambda e: e.dma_start(out=badt[:], in_=badac[:, :]), "c5", writes=[badt])
    op("act", lambda e: e.activation(out=cact[:], in_=cc_t[:], func=AF.Silu), reads=[cc_t], writes=[cact])
    wada_v = w_ada.rearrange("(k p) c -> p k c", p=128)
    b0, pm, pmk = pbank(1)
    for q in range(12):
        wt = wada[q % 2]
        dma("sp", lambda e, wt=wt, q=q: e.dma_start(out=wt[:], in_=wada_v[:, :, q * 512:(q + 1) * 512]),
            ("wada", q % 2), writes=[wt])
        for jj in range(4):
            def mm(e, wt=wt, q=q, jj=jj):
                for k in range(8):
                    ins = e.matmul(pm[:, q * 4 + jj:q * 4 + jj + 1], lhsT=wt[:, k, jj * 128:(jj + 1) * 128],
                                   rhs=cact[:, k:k + 1], start=(k == 0), stop=(k == 7))
                return ins
            op("pe", mm, reads=[wt, cact], writes=pmk)
    op("dve", lambda e: e.tensor_tensor(out=modc[:], in0=pm[:, 0:48], in1=badt[:], op=ALU.add),
       reads=pmk + [badt], writes=[modc])
    op("dve", lambda e: e.scalar_tensor_tensor(out=G12[:, 0:8], in0=modc[:, 8:16], scalar=1.0, in1=n12[:, 0:8],
                                               op0=ALU.add, op1=ALU.mult), reads=[modc, n12], writes=[G12])
    op("dve", lambda e: e.scalar_tensor_tensor(out=G12[:, 8:16], in0=modc[:, 32:40], scalar=1.0, in1=n12[:, 8:16],
                                               op0=ALU.add, op1=ALU.mult), reads=[modc, n12, G12], writes=[G12])

    def gate_bc(col0, dst, gbt):
        op("dve", lambda e: e.tensor_copy(out=gbt[:], in_=bcl(modc[:, col0:col0 + 8], [128, 8, 128])), reads=[modc], writes=[gbt])
        b_, pg, pgk = pbank(2)

        def mmg(e, pg=pg):
            for j in range(8):
                ins = e.matmul(pg[:, j * 128:(j + 1) * 128], lhsT=gbt[:, j, :], rhs=ident_f, start=True, stop=True)
            return ins
        op("pe", mmg, reads=[gbt, cf], writes=pgk)
        op("act", lambda e, pg=pg: e.activation(out=dst[:], in_=pg, func=AF.Copy), reads=pgk, writes=[dst])
    for k in range(8):
        dma("pool", lambda e, k=k: e.dma_start(out=win_bf[:, k, :], in_=w_in[k * 128:(k + 1) * 128, :]), "win", writes=[(win_bf, k)])
    for i in range(3):
        dma("pool", lambda e, i=i: e.dma_start(out=bias3[32 * i:32 * i + 1, :], in_=b_in[:, i * 1024:(i + 1) * 1024]), "binb", writes=[bias3])
    dma("sp", lambda e: e.dma_start(out=bxbc_t[:], in_=bxbc[:, :]), "c6", writes=[bxbc_t])
    dma("sp", lambda e: e.dma_start(out=convw_t[:], in_=convw[:, :, :]), "c7", writes=[convw_t])
    dma("sp", lambda e: e.dma_start(out=convb_t[:], in_=convb[:, :]), "c8", writes=[convb_t])
    dma("sp", lambda e: e.dma_start(out=tmp32[:, 0:64], in_=ssdv.partition_broadcast(128)), "c10", writes=[tmp32])
    dma("sp", lambda e: e.dma_start(out=dtb_bc[:], in_=b_in[:, 4608:4640].partition_broadcast(128)), "c11", writes=[dtb_bc])
    op("dve", lambda e: e.tensor_tensor(out=dtb_bc[:], in0=dtb_bc[:], in1=tmp32[:, 32:64], op=ALU.add),
       reads=[dtb_bc, tmp32], writes=[dtb_bc])
    op("act", lambda e: e.activation(out=a_bc[:], in_=tmp32[:, 0:32], func=AF.Exp), reads=[tmp32], writes=[a_bc])
    op("dve", lambda e: e.tensor_scalar(out=a_bc[:], in0=a_bc[:], scalar1=-1.0, scalar2=None, op0=ALU.mult),
       reads=[a_bc], writes=[a_bc])
    dump("modc", modc[:], [modc])
    P.flush()
    ph0.close()

    P.scope = ph12
    xin = [P.sbuf("xin%d" % i, [128, D], F32) for i in range(2)]
    xnb = [P.sbuf("xnb%d" % i, [128, D], BF16) for i in range(1)]
    hTs = [P.sbuf("hTt%d" % i, [128, 8, TL], BF16) for i in range(2)]
    h2c = P.sbuf("h2c", [128, 8, 2], BF16)
    st4 = [P.sbuf("st4_%d" % i, [128, 4], F32) for i in range(3)]
    pres = [P.sbuf("pre%d" % i, [128, 12, TL + 4], BF16) for i in range(2)]

    def curbuf():
        tb = getattr(P._tl, "tb", 0)
        return hTs[tb], pres[tb]
    hsave = P.sbuf("hsave", [128, 12, 2], BF16)
    dgall = P.sbuf("dgall", [128, 12, 5, 128], BF16)
    xs_tok = [P.sbuf("xs_tok%d" % i, [128, 16, 64], BF16) for i in range(2)]
    B_tok = [P.sbuf("B_tok%d" % i, [128, 256], BF16) for i in range(2)]
    dts = [P.sbuf("dts%d" % i, [128, 8, 32], F32) for i in range(2)]
    xw = [P.sbuf("xw%d" % i, [128, 16, 64], BF16) for i in range(1)]
    carry = P.sbuf("carry", [128, 16, 64], F32)
    carry_bf = P.sbuf("carrybf", [128, D], BF16)
    gt1 = P.sbuf("gt1", [128, D], F32)
    ctmp = gt1[:].rearrange("p (h d) -> p h d", d=64)
    cnt = {"x": 0, "xn": 0, "st": 0, "ca": 0, "ch": 0, "dg": 0}

    for j in range(12):
        for kk in range(5):
            op("dve", lambda e, j=j, kk=kk: e.tensor_scalar(out=dgall[:, j, kk, :], in0=ident_f, scalar1=convw_t[:, j, kk:kk + 1],
                                                           scalar2=None, op0=ALU.mult), reads=[cf, convw_t], writes=[dgall])

    def rot(lst, key):
        i = cnt[key]
        cnt[key] += 1
        return lst[i % len(lst)]

    def rstd_ops(ss_ap, out_ap, n, keys):
        op("act", lambda e: e.activation(out=out_ap, in_=ss_ap, func=AF.Ln, bias=EPS, scale=1.0 / n), reads=keys, writes=keys)
        op("act", lambda e: e.activation(out=out_ap, in_=out_ap, func=AF.Exp, scale=-0.5), reads=keys, writes=keys)

    def norm_T(src, r0, np_, Gc, SHc, dst_fn, dkeys):
        xt = rot(xin, "x")
        dma("sp", lambda e: e.dma_start(out=xt[0:np_, :], in_=src[r0:r0 + np_, :]), xt, writes=[xt])
        s4 = rot(st4, "st")
        op("act", lambda e: e.activation(out=junk[0:np_, :], in_=xt[0:np_, :], func=AF.Square, accum_out=s4[0:np_, 0:1]),
           reads=[xt], writes=[s4, "junk"])
        rstd_ops(s4[0:np_, 0:1], s4[0:np_, 1:2], D, [s4])
        xn = rot(xnb, "xn")
        op("dve", lambda e: e.tensor_scalar(out=xn[0:np_, :], in0=xt[0:np_, :], scalar1=s4[0:np_, 1:2], scalar2=None,
                                            op0=ALU.mult), reads=[xt, s4], writes=[xn])
        b_, pt, ptk = pbank(1)
        ptb = pt.bitcast(BF16)

        def tr(e):
            for k in range(8):
                ins = e.transpose(out=ptb[:, k * np_:(k + 1) * np_], in_=xn[0:np_, k * 128:(k + 1) * 128],
                                  identity=ident_bf[0:np_, 0:np_])
            return ins
        op("pe", tr, reads=[xn, cbf], writes=ptk)
        dst = dst_fn(None)
        op("dve", lambda e: e.tensor_tensor(out=dst, in0=ptb[:, 0:8 * np_].rearrange("p (k t) -> p k t", t=np_),
                                            in1=bcl(Gc, [128, 8, np_]), op=ALU.mult), reads=ptk + [G12], writes=dkeys)
        op("dve", lambda e: e.tensor_tensor(out=dst, in0=dst, in1=bcl(SHc, [128, 8, np_]), op=ALU.add),
           reads=dkeys + [modc], writes=dkeys)

    def build_hT(t, src, Gc, SHc):
        hTt, pre = curbuf()
        for c4 in range(CPT):
            norm_T(src, (t * CPT + c4) * 128, 128, Gc, SHc, lambda k, c4=c4: hTt[:, :, c4 * 128:(c4 + 1) * 128], [(hTt, c4)])

    def proj_fm(t, blocks, reverse):
        hTt, pre = curbuf()
        tn = t - 1 if reverse else t + 1
        tp = t + 1 if reverse else t - 1
        far = slice(0, 2) if reverse else slice(TL + 2, TL + 4)
        near = slice(TL + 2, TL + 4) if reverse else slice(0, 2)
        src_near = slice(2, 4) if reverse else slice(TL, TL + 2)
        nb_ = len(blocks)
        PK = [(pre, j) for j in blocks]
        if 0 <= tn < NT:
            rr = t * TL - 2 if reverse else (t + 1) * TL
            norm_T(x, rr, 2, G12[:, 0:8], modc[:, 0:8], lambda k: h2c[:, :, :], [h2c])
        for j in blocks:
            c0 = 3072 + j * 128
            b_, pp, ppk = pbank(1)

            def mm(e, pp=pp, c0=c0):
                for k in range(8):
                    ins = e.matmul(pp[:, 0:TL], lhsT=win_bf[:, k, c0:c0 + 128], rhs=hTt[:, k, :], start=(k == 0), stop=(k == 7))
                return ins
            op("pe", mm, reads=[(win_bf, k) for k in range(8)] + [(hTt, c) for c in range(CPT)], writes=ppk)
            op("act", lambda e, pp=pp, j=j: e.activation(out=pre[:, j, 2:2 + TL], in_=pp[:, 0:TL], func=AF.Identity,
                                                        bias=bxbc_t[:, j:j + 1], scale=1.0),
               reads=ppk + [bxbc_t], writes=[(pre, j)])
        if 0 <= tn < NT:
            b_, ph_, phk = pbank(1)

            def mmh(e, ph_=ph_):
                for j in blocks:
                    c0 = 3072 + j * 128
                    for k in range(8):
                        ins = e.matmul(ph_[:, j * 2:j * 2 + 2], lhsT=win_bf[:, k, c0:c0 + 128], rhs=h2c[:, k, :],
                                       start=(k == 0), stop=(k == 7))
                return ins
            op("pe", mmh, reads=[(win_bf, k) for k in range(8)] + [h2c], writes=phk)
            op("dve", lambda e, ph_=ph_: e.tensor_tensor(
                out=pre[:, 0:nb_, far], in0=ph_[:, 0:2 * nb_].rearrange("p (j c) -> p j c", c=2),
                in1=bcl(bxbc_t[:, 0:nb_], [128, nb_, 2]), op=ALU.add),
                reads=phk + [bxbc_t], writes=PK)
        else:
            op("dve", lambda e: e.memset(pre[:, 0:nb_, far], 0.0), writes=PK)
        if 0 <= tp < NT:
            op("dve", lambda e: e.tensor_copy(out=pre[:, 0:nb_, near], in_=hsave[:, 0:nb_, :]), reads=[hsave], writes=PK)
        else:
            op("dve", lambda e: e.memset(pre[:, 0:nb_, near], 0.0), writes=PK)
        op("dve", lambda e: e.tensor_copy(out=hsave[:, 0:nb_, :], in_=pre[:, 0:nb_, src_near]), reads=PK, writes=[hsave])

    def conv_silu(t, blocks):
        hTt, pre = curbuf()
        for j in blocks:
            b_, pcv, pcvk = pbank(1)

            def mmc(e, j=j, pcv=pcv):
                for kk in range(5):
                    ins = e.matmul(pcv[:, 0:TL], lhsT=dgall[:, j, kk, :], rhs=pre[:, j, kk:kk + TL], start=(kk == 0), stop=(kk == 4))
                return ins
            op("pe", mmc, reads=[dgall, (pre, j)], writes=pcvk)
            op("act", lambda e, pcv=pcv, j=j: e.activation(out=pre[:, j, 2:2 + TL], in_=pcv[:, 0:TL], func=AF.Silu,
                                                          bias=convb_t[:, j:j + 1], scale=1.0),
               reads=pcvk + [convb_t], writes=[(pre, j)])

    def to_tok(c4, par):
        hTt, pre = curbuf()
        b_, pt, ptk = pbank(1)
        ptb = pt.bitcast(BF16)

        def tr(e, ptb=ptb):
            for j in range(8):
                ins = e.transpose(out=ptb[:, j * 128:(j + 1) * 128], in_=pre[:, j, 2 + c4 * 128:2 + (c4 + 1) * 128], identity=ident_bf)
            return ins
        op("pe", tr, reads=[(pre, j) for j in range(8)] + [cbf], writes=ptk)
        op("act", lambda e, ptb=ptb: e.activation(out=xs_tok[par][:].rearrange("p h d -> p (h d)"), in_=ptb, func=AF.Copy),
           reads=ptk, writes=[xs_tok[par]])
        b2, pt2, pt2k = pbank(1)
        pt2b = pt2.bitcast(BF16)

        def tr2(e, pt2b=pt2b):
            for g in range(2):
                ins = e.transpose(out=pt2b[:, g * 128:(g + 1) * 128], in_=pre[:, 8 + g, 2 + c4 * 128:2 + (c4 + 1) * 128], identity=ident_bf)
            return ins
        op("pe", tr2, reads=[(pre, 8), (pre, 9), cbf], writes=pt2k)
        op("dve", lambda e, pt2b=pt2b: e.tensor_copy(out=B_tok[par][:], in_=pt2b[:, 0:256]), reads=pt2k, writes=[B_tok[par]])

    def dt_proj(c4, dd):
        hTt, pre = curbuf()
        b_, pd, pdk = pbank(1)

        def mm2(e, pd=pd):
            for k in range(8):
                ins = e.matmul(pd[:, 0:32], lhsT=hTt[:, k, c4 * 128:(c4 + 1) * 128], rhs=win_bf[:, k, 4608:4640],
                               start=(k == 0), stop=(k == 7))
            return ins
        op("pe", mm2, reads=[(win_bf, k) for k in range(8)] + [(hTt, c4)], writes=pdk)
        v, m, na = dd[:, 2, :], dd[:, 3, :], dd[:, 4, :]
        op("dve", lambda e: e.tensor_tensor(out=v, in0=pd[:, 0:32], in1=dtb_bc[:], op=ALU.add), reads=pdk + [dtb_bc], writes=[dd])
        op("dve", lambda e: e.tensor_scalar(out=m, in0=v, scalar1=0.0, scalar2=None, op0=ALU.max), reads=[dd], writes=[dd])
        op("dve", lambda e: e.scalar_tensor_tensor(out=na, in0=m, scalar=-2.0, in1=v, op0=ALU.mult, op1=ALU.add), reads=[dd], writes=[dd])
        op("act", lambda e: e.activation(out=na, in_=na, func=AF.Exp), reads=[dd], writes=[dd])
        op("act", lambda e: e.activation(out=na, in_=na, func=AF.Ln, bias=1.0, scale=1.0), reads=[dd], writes=[dd])
        op("dve", lambda e: e.tensor_tensor(out=dd[:, 0, :], in0=m, in1=na, op=ALU.add), reads=[dd], writes=[dd])
        op("dve", lambda e: e.tensor_tensor(out=dd[:, 1, :], in0=dd[:, 0, :], in1=a_bc[:], op=ALU.mult), reads=[dd, a_bc], writes=[dd])

    def state_update(dd, hs, tri_f, first, par=0):
        b_, pq, pqk = pbank(1)
        op("pe", lambda e: e.matmul(pq[:, 0:16], lhsT=tri_f, rhs=dd[:, 1, hs], start=True, stop=True), reads=[dd, cf], writes=pqk)
        op("pe", lambda e: e.matmul(pq[:, 16:32], lhsT=ones_f, rhs=dd[:, 1, hs], start=True, stop=True), reads=[dd, cf] + pqk, writes=pqk)
        wv, dec = dd[:, 5, 0:16], dd[:, 5, 16:32]
        op("act", lambda e: e.activation(out=dd[:, 5, :], in_=pq[:, 0:32], func=AF.Exp), reads=pqk, writes=[dd])
        op("dve", lambda e: e.tensor_tensor(out=wv, in0=wv, in1=dd[:, 0, hs], op=ALU.mult), reads=[dd], writes=[dd])
        xwt = xw[0]
        op("dve", lambda e: e.tensor_tensor(out=xwt[:], in0=xs_tok[par][:], in1=bcl(wv, [128, 16, 64]), op=ALU.mult),
           reads=[xs_tok[par], dd], writes=[xwt])
        b2, psS, psk = pbank(2)

        def mm(e):
            for g in range(2):
                ins = e.matmul(psS[:, g * 512:(g + 1) * 512], lhsT=B_tok[par][:, g * 128:(g + 1) * 128],
                               rhs=xwt[:, g * 8:(g + 1) * 8, :].rearrange("p h d -> p (h d)"), start=True, stop=True)
            return ins
        op("pe", mm, reads=[B_tok[par], xwt], writes=psk)
        crf = carry[:].rearrange("p h d -> p (h d)")
        if first:
            op("dve", lambda e: e.tensor_copy(out=crf, in_=psS), reads=psk, writes=[carry])
        else:
            op("dve", lambda e: e.tensor_tensor(out=carry[:], in0=carry[:], in1=bcl(dec, [128, 16, 64]), op=ALU.mult),
               reads=[carry, dd], writes=[carry])
            op("dve", lambda e: e.tensor_tensor(out=crf, in0=crf, in1=psS, op=ALU.add),
               reads=[carry] + psk, writes=[carry])
        op("act", lambda e: e.activation(out=carry_bf[:], in_=crf, func=AF.Copy), reads=[carry], writes=[carry_bf])

    ph1x = contextlib.ExitStack()
    P.scope = ph1x
    wstg = [P.sbuf("wstg%d" % i, [128, 4096], BF16) for i in range(3)]
    P.scope = ph12
    wsrc = [(w13p.rearrange("(n p r) c -> n p (r c)", p=128, r=2), w13c_d.rearrange("(n p r) c -> n p (r c)", p=128, r=2), 32),
            (w2p.rearrange("(n p r) c -> n p (r c)", p=128, r=2), w2c_d.rearrange("(n p r) c -> n p (r c)", p=128, r=2), 16)]
    wjobs = [(sv, dv, n) for (sv, dv, cnt_) in wsrc for n in range(cnt_)]
    wpos = {"ld": 0, "st": 0}

    def wconv_step(nsteps):
        for _ in range(nsteps):
            if wpos["ld"] < len(wjobs):
                sv, dv, n = wjobs[wpos["ld"]]
                wb = wstg[wpos["ld"] % 3]
                dma("pool", lambda e, sv=sv, n=n, wb=wb: e.dma_start(out=wb[:], in_=sv[n]), wb, writes=[wb])
                wpos["ld"] += 1
            if wpos["ld"] - wpos["st"] >= 2 or (wpos["ld"] == len(wjobs) and wpos["st"] < len(wjobs)):
                sv, dv, n = wjobs[wpos["st"]]
                wb = wstg[wpos["st"] % 3]
                dma("pool", lambda e, dv=dv, n=n, wb=wb: e.dma_start(out=dv[n], in_=wb[:]), ("wst", wpos["st"] % 3), reads=[wb])
                wpos["st"] += 1
    XSB = list(range(10))
    op("dve", lambda e: e.memset(carry_bf[:], 0.0), writes=[carry_bf])

    def prep1(t):
        P._tl.tb = t % 2
        build_hT(t, x, G12[:, 0:8], modc[:, 0:8])
        proj_fm(t, XSB, True)
        wconv_step(2)
        conv_silu(t, XSB)
        wconv_step(1)

    def chunks1(t):
        P._tl.tb = t % 2
        for c4 in range(CPT - 1, -1, -1):
            c = t * CPT + c4
            dd = dts[c % 2]
            dma("sp", lambda e, c=c: e.dma_start(out=prevb_d[c, :, :], in_=carry_bf[:]), "prevb_st", reads=[carry_bf])
            to_tok(c4, 0)
            dt_proj(c4, dd)
            state_update(dd, slice(16, 32), SL_f, first=(c == NCH - 1))
    prep1(NT - 1)
    for t in range(NT - 1, -1, -1):
        fns = [lambda t=t: chunks1(t)]
        qs = [3]
        if t > 0:
            fns.append(lambda t=t: prep1(t - 1))
            qs.append(4)
        P.interleave(fns, qs)
    P._tl.tb = 0
    wconv_step(len(wjobs) + 2)
    assert wpos["st"] == len(wjobs)
    P.flush()
    ph1x.close()

    ph2 = contextlib.ExitStack()
    P.scope = ph2
    lng_bc = P.sbuf("lng_bc", [128, D], BF16)
    CB = P.sbuf("CB", [128, 8, 128], F32)
    wsT_bf = P.sbuf("wsT_bf", [128, 8, 128], BF16)
    rsw = P.sbuf("rsw", [128, 16], F32)
    Ddiag = P.sbuf("Ddiag", [128, 8, 128], BF16)
    dsk_t = P.sbuf("dsk_t", [128, 8], F32)
    SGt = P.sbuf("SGt", [128, 128], F32)
    bs_t = P.sbuf("bs_t", [128, 8], F32)
    guv = P.sbuf("guv", [128, 2 * D], BF16)
    vnb = P.sbuf("vnb", [128, D], BF16)
    yab = vnb
    yabS = P.sbuf("yabS", [128, D], BF16)
    hib = P.sbuf("hib", [128, 32], BF16)
    yTa = [P.sbuf("yTa%d" % i, [128, 8, 128], BF16) for i in range(2)]
    yTb = P.sbuf("yTb", [128, 8, 128], BF16)
    zss = [P.sbuf("zs%d" % i, [128, D], BF16) for i in range(2)]
    Ef = P.sbuf("Ef", [128, 16, 128], BF16)
    Eb = P.sbuf("Eb", [128, 16, 128], BF16)
    scT = P.sbuf("scT", [128, 2, 128], BF16)
    pvb = [P.sbuf("pvb%d" % i, [128, D], BF16) for i in range(1)]
    y1 = P.sbuf("y1", [128, 16, 64], F32)
    y1b = y1[:].rearrange("p h d -> p (h d)").bitcast(BF16)
    rhsb = [y1b[:, i * D:(i + 1) * D].rearrange("p (h l) -> p h l", l=128) for i in range(2)]
    rhsb2 = [xw[0][:].rearrange("p h d -> p (h d)").rearrange("p (h l) -> p h l", l=128),
             yabS[:].rearrange("p (h l) -> p h l", l=128)]
    RB = [(rhsb, [(y1, 0), (y1, 1)]), (rhsb2, [xw[0], yabS])]
    sm = [P.sbuf("sm%d" % i, [128, 16], F32) for i in range(2)]
    wtmp2 = y1[:].rearrange("p h d -> p (h d)").rearrange("p (h d) -> p h d", d=128)
    lnb_bc = gt1
    dma("pool", lambda e: e.dma_start(out=lng_bc[:], in_=lngb[0:1, :].partition_broadcast(128)), "p2a", writes=[lng_bc])
    dma("sp", lambda e: e.dma_start(out=lnb_bc[:], in_=lngb[1:2, :].partition_broadcast(128)), "p2b", writes=[gt1])
    dma("sp", lambda e: e.dma_start(out=wtmp2, in_=w_sT[:, :, :]), "p2c", writes=[y1])
    dma("sp", lambda e: e.dma_start(out=bs_t[:], in_=b_s2[:, :]), "p2d", writes=[bs_t])
    dma("sp", lambda e: e.dma_start(out=dsk_t[:], in_=dskc[:, :]), "p2e", writes=[dsk_t])
    op("act", lambda e: e.activation(out=wsT_bf[:], in_=wtmp2, func=AF.Copy), reads=[y1], writes=[wsT_bf])
    b_, prs, prsk = pbank(1)

    def mmrs(e):
        for h in range(8):
            ins = e.matmul(prs[:, h:h + 1], lhsT=wtmp2[:, h, :], rhs=ones_f[:, 0:1], start=True, stop=True)
        return ins
    op("pe", mmrs, reads=[y1, cf], writes=prsk)
    op("dve", lambda e: e.tensor_copy(out=rsw[:, 0:8], in_=prs[:, 0:8]), reads=prsk, writes=[rsw])
    op("dve", lambda e: e.tensor_tensor(out=CB[:], in0=lnb_bc[:].rearrange("p (h d) -> p h d", d=128),
                                        in1=bcl(rsw[:, 0:8], [128, 8, 128]), op=ALU.mult), reads=[gt1, rsw], writes=[CB])
    op("dve", lambda e: e.tensor_tensor(out=CB[:], in0=CB[:], in1=bcl(bs_t[:], [128, 8, 128]), op=ALU.add),
       reads=[CB, bs_t], writes=[CB])
    for j in range(8):
        op("dve", lambda e, j=j: e.tensor_scalar(out=Ddiag[:, j, :], in0=ident_f, scalar1=dsk_t[:, j:j + 1], scalar2=None,
                                                op0=ALU.mult), reads=[cf, dsk_t], writes=[Ddiag])
    SG_f = SGt[:]
    op("dve", lambda e: e.tensor_tensor(out=SGt[:], in0=L_f, in1=ident_f, op=ALU.subtract), reads=[cf], writes=[SGt])
    P.flush()
    ALLB = list(range(12))
    wink = [(win_bf, k) for k in range(8)]

    def tok_proj(c4, col0, nblk, pz):
        hTt, pre = curbuf()
        def mm(e):
            for k in range(8):
                for n in range(nblk):
                    c0 = col0 + n * 512
                    e.matmul(pz[:, n * 512:(n + 1) * 512], lhsT=hTt[:, k, c4 * 128:(c4 + 1) * 128],
                             rhs=win_bf[:, k, c0:c0 + 512], start=(k == 0), stop=False)
            for n in range(nblk):
                c0 = col0 + n * 512
                r = 32 * (c0 // 1024)
                ins = e.matmul(pz[:, n * 512:(n + 1) * 512], lhsT=ones_bf[r:r + 1, :], rhs=bias3[r:r + 1, c0 % 1024:c0 % 1024 + 512],
                               start=False, stop=True)
            return ins
        return mm

    def gmlp_chunk(c4, ya):
        hTt, pre = curbuf()
        g = guv
        s = sm[0]
        for half in range(2):
            b_, pu, puk = pbank(2)
            op("pe", tok_proj(c4, half * 1024, 2, pu), reads=wink + [(hTt, c4), bias3, cbf], writes=puk)
            if half == 0:
                op("act", lambda e, pu=pu: e.activation(out=g[:, 0:1024], in_=pu, func=AF.Gelu), reads=puk, writes=[(g, 0)])
            else:
                op("act", lambda e, pu=pu: e.activation(out=g[:, 1024:2048], in_=pu, func=AF.Gelu, accum_out=s[:, 0:1]),
                   reads=puk, writes=[(g, 1), s])
        v = g[:, 1024:2048]
        op("act", lambda e: e.activation(out=junk[:], in_=v, func=AF.Square, accum_out=s[:, 1:2]), reads=[(g, 1), s], writes=[s, "junk"])
        op("dve", lambda e: e.tensor_scalar(out=s[:, 2:3], in0=s[:, 0:1], scalar1=1.0 / 1024, scalar2=None, op0=ALU.mult), reads=[s], writes=[s])
        op("dve", lambda e: e.tensor_tensor(out=s[:, 3:4], in0=s[:, 2:3], in1=s[:, 2:3], op=ALU.mult), reads=[s], writes=[s])
        op("dve", lambda e: e.scalar_tensor_tensor(out=s[:, 4:5], in0=s[:, 1:2], scalar=1.0 / 1024, in1=s[:, 3:4],
                                                   op0=ALU.mult, op1=ALU.subtract), reads=[s], writes=[s])
        op("act", lambda e: e.activation(out=s[:, 5:6], in_=s[:, 4:5], func=AF.Ln, bias=EPS, scale=1.0), reads=[s], writes=[s])
        op("act", lambda e: e.activation(out=s[:, 5:6], in_=s[:, 5:6], func=AF.Exp, scale=-0.5), reads=[s], writes=[s])
        op("dve", lambda e: e.scalar_tensor_tensor(out=s[:, 6:7], in0=s[:, 2:3], scalar=-1.0, in1=s[:, 5:6],
                                                   op0=ALU.mult, op1=ALU.mult), reads=[s], writes=[s])
        op("dve", lambda e: e.tensor_scalar(out=vnb[:], in0=v, scalar1=s[:, 5:6], scalar2=s[:, 6:7], op0=ALU.mult, op1=ALU.add),
           reads=[(g, 1), s], writes=[vnb])
        b_, pmx, pmxk = pbank(2)

        def mms(e, pmx=pmx):
            for hh in range(8):
                ins = e.matmul(pmx[:, hh * 128:(hh + 1) * 128], lhsT=wsT_bf[:, hh, :], rhs=vnb[:, hh * 128:(hh + 1) * 128],
                               start=True, stop=True)
            return ins
        op("pe", mms, reads=[wsT_bf, vnb], writes=pmxk)
        op("dve", lambda e, pmx=pmx: e.tensor_tensor(out=gt1[:], in0=pmx, in1=lng_bc[:], op=ALU.mult), reads=pmxk + [lng_bc], writes=[gt1])
        op("dve", lambda e: e.tensor_tensor(out=gt1[:], in0=gt1[:], in1=CB[:].rearrange("p h d -> p (h d)"), op=ALU.add),
           reads=[gt1, CB], writes=[gt1])
        op("dve", lambda e: e.tensor_tensor(out=gt1[:], in0=gt1[:], in1=g[:, 0:1024], op=ALU.mult), reads=[gt1, (g, 0)], writes=[gt1])
        op("act", lambda e: e.activation(out=junk[:], in_=gt1[:], func=AF.Square, accum_out=s[:, 7:8]), reads=[gt1, s], writes=[s, "junk"])
        rstd_ops(s[:, 7:8], s[:, 8:9], D, [s])
        op("act", lambda e: e.activation(out=yab[:], in_=gt1[:], func=AF.Copy, scale=s[:, 8:9]), reads=[gt1, s], writes=[yab])
        b_, pt, ptk = pbank(1)
        ptb = pt.bitcast(BF16)

        def tr(e, ptb=ptb):
            for k in range(8):
                ins = e.transpose(out=ptb[:, k * 128:(k + 1) * 128], in_=yab[:, k * 128:(k + 1) * 128], identity=ident_bf)
            return ins
        op("pe", tr, reads=[yab, cbf], writes=ptk)
        op("dve", lambda e, ptb=ptb: e.tensor_copy(out=yTa[ya][:].rearrange("p k t -> p (k t)"), in_=ptb), reads=ptk, writes=[yTa[ya]])

    def z_chunk(c4, par):
        hTt, pre = curbuf()
        zs = zss[par]
        b_, pz, pzk = pbank(2)
        op("pe", tok_proj(c4, 2048, 2, pz), reads=wink + [(hTt, c4), bias3, cbf], writes=pzk)
        op("act", lambda e, pz=pz: e.activation(out=zs[:], in_=pz, func=AF.Silu), reads=pzk, writes=[zs])

    def decay_prep(dd):
        b_, pc, pck = pbank(1)
        op("pe", lambda e: e.matmul(pc[:, 0:16], lhsT=U_f, rhs=dd[:, 1, 0:16], start=True, stop=True), reads=[dd, cf], writes=pck)
        op("pe", lambda e: e.matmul(pc[:, 16:32], lhsT=L_f, rhs=dd[:, 1, 16:32], start=True, stop=True), reads=[dd, cf] + pck, writes=pck)
        op("dve", lambda e: e.tensor_copy(out=dd[:, 6, :], in_=pc[:, 0:32]), reads=pck, writes=[dd])
        op("act", lambda e: e.activation(out=dd[:, 7, :], in_=dd[:, 0, :], func=AF.Ln), reads=[dd], writes=[dd])
        op("dve", lambda e: e.tensor_tensor(out=dd[:, 7, :], in0=dd[:, 7, :], in1=dd[:, 6, :], op=ALU.subtract), reads=[dd], writes=[dd])
        op("dve", lambda e: e.tensor_copy(out=hib[:], in_=dd[:, 1, :]), reads=[dd], writes=[hib])
        op("dve", lambda e: e.tensor_copy(out=dd[:, 2, :], in_=hib[:]), reads=[hib], writes=[dd])
        op("dve", lambda e: e.tensor_tensor(out=dd[:, 3, :], in0=dd[:, 1, :], in1=dd[:, 2, :], op=ALU.subtract), reads=[dd], writes=[dd])

    def decay_stages(dd):
        stage = 0
        for q in range(2):
            for (h0, tri_bf, mask_bf, Eout) in ((0, U_bf, mF_bf, Ef), (16, L_bf, mB_bf, Eb)):
                rb, rbk = RB[stage % 2]
                stage += 1
                for i in range(2):
                    op("dve", lambda e, i=i, q=q, h0=h0, tri_bf=tri_bf, rb=rb: e.tensor_tensor(
                        out=rb[i], in0=bcm(tri_bf, [128, 8, 128]),
                        in1=bcl(dd[:, 2 + i, h0 + q * 8:h0 + (q + 1) * 8], [128, 8, 128]), op=ALU.mult),
                       reads=[dd, cbf], writes=[rbk[i]])
                b_, pa, pak = pbank(2)

                def mm(e, pa=pa, mask_bf=mask_bf, rb=rb):
                    for n in range(2):
                        hh = n * 4
                        o_ = pa[:, n * 512:(n + 1) * 512]
                        e.matmul(o_, lhsT=ones_bf, rhs=rb[0][:, hh:hh + 4, :].rearrange("p h l -> p (h l)"), start=True, stop=False)
                        e.matmul(o_, lhsT=ones_bf, rhs=rb[1][:, hh:hh + 4, :].rearrange("p h l -> p (h l)"), start=False, stop=False)
                        for h4 in range(4):
                            ins = e.matmul(o_[:, h4 * 128:(h4 + 1) * 128], lhsT=ident_bf, rhs=mask_bf, start=False, stop=(h4 == 3))
                    return ins
                op("pe", mm, reads=rbk + [cbf], writes=pak)
                for h8 in range(8):
                    hh = q * 8 + h8
                    hsel = h0 + hh
                    op("act", lambda e, pa=pa, h8=h8, hh=hh, hsel=hsel, Eout=Eout: e.activation(
                        out=Eout[:, hh, :], in_=pa[:, h8 * 128:(h8 + 1) * 128], func=AF.Exp, bias=dd[:, 7, hsel:hsel + 1], scale=1.0),
                        reads=pak + [dd], writes=[(Eout, hh)])

    def ssd_chunk(t, c4, c, par):
        hTt, pre = curbuf()
        dd = dts[par]
        zs = zss[par]
        pv = pvb[0]
        dma("sp", lambda e, pv=pv, c=c: e.dma_start(out=pv[:], in_=prevb_d[c, :, :]), pv, writes=[pv])
        decay_stages(dd)
        EFK = [(Ef, h) for h in range(16)]
        EBK = [(Eb, h) for h in range(16)]
        op("dve", lambda e: e.tensor_tensor(out=Ef[:], in0=Ef[:], in1=Eb[:], op=ALU.add), reads=EFK + EBK, writes=EFK)
        b_, psc, psck = pbank(1)

        def mmsc(e, psc=psc):
            for g in range(2):
                ins = e.matmul(psc[:, g * 128:(g + 1) * 128], lhsT=pre[:, 8 + g, 2 + c4 * 128:2 + (c4 + 1) * 128],
                               rhs=pre[:, 10 + g, 2 + c4 * 128:2 + (c4 + 1) * 128], start=True, stop=True)
            return ins
        op("pe", mmsc, reads=[(pre, j) for j in range(8, 12)], writes=psck)
        op("act", lambda e, psc=psc: e.activation(out=scT[:].rearrange("p g l -> p (g l)"), in_=psc[:, 0:256], func=AF.Copy),
           reads=psck, writes=[scT])
        Mt = Eb
        for g in range(2):
            op("dve", lambda e, g=g: e.tensor_tensor(
                out=Mt[:, g * 8:(g + 1) * 8, :], in0=Ef[:, g * 8:(g + 1) * 8, :], in1=bcm(scT[:, g, :], [128, 8, 128]), op=ALU.mult),
               reads=EFK + [scT], writes=[(Eb, h) for h in range(g * 8, g * 8 + 8)])
        xst = xs_tok[par]
        Y1K = [(y1, 0), (y1, 1)]
        CT = [pre[:, 10 + g, 2 + c4 * 128:2 + (c4 + 1) * 128] for g in range(2)]
        op("act", lambda e: e.activation(out=dd[:, 5, :], in_=dd[:, 6, :], func=AF.Exp), reads=[dd], writes=[dd])
        yv = y1[:].rearrange("p h d -> p (h d)")

        def yoff(st):
            b_, po_, pok = pbank(2)

            def mmo(e, po_=po_, st=st):
                for g in range(2):
                    ins = e.matmul(po_[:, g * 512:(g + 1) * 512], lhsT=CT[g], rhs=st[:, g * 512:(g + 1) * 512], start=True, stop=True)
                return ins
            op("pe", mmo, reads=[(pre, 10), (pre, 11), st], writes=pok)
            return po_, pok
        have_f = c > 0
        have_b = c < NCH - 1
        if have_f:
            po_, pok = yoff(carry_bf)
            op("dve", lambda e, po_=po_: e.tensor_tensor(out=y1[:], in0=po_.rearrange("p (h d) -> p h d", d=64),
                                                         in1=bcl(dd[:, 5, 0:16], [128, 16, 64]), op=ALU.mult), reads=pok + [dd], writes=Y1K)
        b_, py, pyk = pbank(2)

        def mmy(e, py=py):
            for j in range(8):
                for hh in range(2):
                    h = 2 * j + hh
                    e.matmul(py[:, h * 64:(h + 1) * 64], lhsT=pre[:, j, 2 + c4 * 128:2 + (c4 + 1) * 128],
                             rhs=Ddiag[:, j, hh * 64:(hh + 1) * 64], start=True, stop=False)
                    ins = e.matmul(py[:, h * 64:(h + 1) * 64], lhsT=Mt[:, h, :], rhs=xst[:, h, :], start=False, stop=True)
            return ins
        op("pe", mmy, reads=[(pre, j) for j in range(8)] + [Ddiag, xst] + EBK, writes=pyk)
        if have_f:
            op("dve", lambda e, py=py: e.tensor_tensor(out=yv, in0=py, in1=yv, op=ALU.add), reads=pyk + Y1K, writes=Y1K)
        else:
            op("dve", lambda e, py=py: e.tensor_copy(out=yv, in_=py), reads=pyk, writes=Y1K)
        if have_b:
            po_, pok = yoff(pv)
            op("dve", lambda e, po_=po_: e.tensor_tensor(out=po_.rearrange("p (h d) -> p h d", d=64), in0=po_.rearrange("p (h d) -> p h d", d=64),
                                                         in1=bcl(dd[:, 5, 16:32], [128, 16, 64]), op=ALU.mult), reads=pok + [dd], writes=pok)
            op("dve", lambda e, po_=po_: e.tensor_tensor(out=yv, in0=po_, in1=yv, op=ALU.add), reads=pok + Y1K, writes=Y1K)
        op("dve", lambda e: e.tensor_tensor(out=yv, in0=yv, in1=zs[:], op=ALU.mult), reads=Y1K + [zs], writes=Y1K)
        s = sm[1]
        for g in range(2):
            op("act", lambda e, g=g: e.activation(out=junk[:, 0:512], in_=yv[:, g * 512:(g + 1) * 512], func=AF.Square,
                                                  accum_out=s[:, g:g + 1]), reads=Y1K + [s], writes=[s, "junk"])
        rstd_ops(s[:, 0:2], s[:, 2:4], 512, [s])
        for g in range(2):
            op("act", lambda e, g=g: e.activation(out=yabS[:, g * 512:(g + 1) * 512], in_=yv[:, g * 512:(g + 1) * 512], func=AF.Copy,
                                                  scale=s[:, 2 + g:3 + g]), reads=Y1K + [s], writes=[yabS])
        b_, pt, ptk = pbank(1)
        ptb = pt.bitcast(BF16)

        def tr(e, ptb=ptb):
            for k in range(8):
                ins = e.transpose(out=ptb[:, k * 128:(k + 1) * 128], in_=yabS[:, k * 128:(k + 1) * 128], identity=ident_bf)
            return ins
        op("pe", tr, reads=[yabS, cbf], writes=ptk)
        op("dve", lambda e, ptb=ptb: e.tensor_copy(out=yTb[:].rearrange("p k t -> p (k t)"), in_=ptb), reads=ptk, writes=[yTb])
        if c < NCH - 1:
            state_update(dd, slice(0, 16), SG_f, first=(c == 0), par=par)

    def outproj_chunk(t, c4, ya):
        c = t * CPT + c4
        dma("sp", lambda e: e.dma_start(out=yT_d[c, :, 0:8, :], in_=yTa[ya][:]), ("yTa_st", ya), reads=[yTa[ya]])
        dma("sp", lambda e: e.dma_start(out=yT_d[c, :, 8:16, :], in_=yTb[:]), "yTb_st", reads=[yTb])

    def prep_tile(t):
        P._tl.tb = t % 2
        build_hT(t, x, G12[:, 0:8], modc[:, 0:8])
        proj_fm(t, ALLB, False)
        conv_silu(t, ALLB)

    def S_stream(c):
        t, c4 = divmod(c, CPT)
        P._tl.tb = t % 2
        ssd_chunk(t, c4, c, c % 2)
        outproj_chunk(t, c4, c % 2)

    def G_stream(c):
        t, c4 = divmod(c, CPT)
        P._tl.tb = t % 2
        par = c % 2
        z_chunk(c4, par)
        to_tok(c4, par)
        dt_proj(c4, dts[par])
        decay_prep(dts[par])
        gmlp_chunk(c4, par)
    prep_tile(0)
    G_stream(0)
    for c in range(NCH):
        t, c4 = divmod(c, CPT)
        fns = [lambda c=c: S_stream(c)]
        qs = [5]
        if c + 1 < NCH:
            fns.append(lambda c=c: G_stream(c + 1))
            qs.append(3)
        else:
            fns.append(lambda: None)
            qs.append(1)
        if c4 == 0 and t + 1 < NT:
            fns.append(lambda t=t: prep_tile(t + 1))
            qs.append(4)
        P.interleave(fns, qs)
    P._tl.tb = 0
    P.flush()
    ph2.close()
    ph12.close()

    ph3 = contextlib.ExitStack()
    P.scope = ph3
    TSL = 256
    NTL = (2 * S) // TSL + 32
    NSLOT = NTL * TSL
    hs_d = nc.dram_tensor("hs_s", [NSLOT, D], BF16, kind=skind).ap()
    ys_d = nc.dram_tensor("ys_s", [NSLOT, D], BF16, kind=skind).ap()
    psn[0] = 0
    gate2_bc = P.sbuf("gate2_bc", [128, D], F32)
    fing_bc = P.sbuf("fing_bc", [128, D], F32)
    G2_bc = P.sbuf("G2_bc", [128, D], F32)
    sh2_bc = P.sbuf("sh2_bc", [128, D], F32)
    c2t = P.sbuf("c2t", [128, 34], F32)
    thr_bc, pcol = c2t[:, 0:32], c2t[:, 32:33]
    h2tok = P.sbuf("h2tok", [128, NCH, D], BF16)
    comb_all = P.sbuf("comb_all", [128, NCH, 32], F32)
    pos_all = P.sbuf("pos_all", [128, NCH, 32], F32)
    w_all = P.sbuf("w_all", [128, NCH, 2], F32)
    idx_all = P.sbuf("idx_all", [128, NCH, 2], mybir.dt.int32)
    runc = P.sbuf("runc", [128, 32], F32)
    segs = P.sbuf("segs", [128, 32], F32)
    wk3 = P.sbuf("wk3", [128, 32, 32], F32)
    sm3 = P.sbuf("sm3", [128, 256], F32)
    IDXW = P.sbuf("IDXW", [128, 128], mybir.dt.int32)
    IDXA = P.sbuf("IDXA", [128, 128], mybir.dt.int32)
    IDXB = P.sbuf("IDXB", [128, 128], mybir.dt.int32)
    h2Tcs = [P.sbuf("h2Tc%d" % i, [128, 8, 128], BF16) for i in range(4)]
    rts = [P.sbuf("rts%d" % i, [128, 128], F32) for i in range(4)]
    ohts = [P.sbuf("oht%d" % i, [128, 32], F32) for i in range(4)]
    cnt_all = P.sbuf("cnt_all", [128, NCH, 32], F32)
    wr_bf = P.sbuf("wr_bf", [128, 8, 36], BF16)
    br_bc = P.sbuf("br_bc", [128, 36], F32)
    rt = P.sbuf("rt", [128, 128], F32)
    xstg = [P.sbuf("xstg%d" % i, [128, D], F32) for i in range(2)]
    ot = [P.sbuf("ot%d" % i, [128, D], F32) for i in range(2)]
    s3 = [P.sbuf("s3_%d" % i, [128, 4], F32) for i in range(4)]
    ph3a = contextlib.ExitStack()
    P.scope = ph3a
    wout3 = P.sbuf("wout3", [128, 16, D], BF16)
    ytc = [P.sbuf("ytc%d" % i, [128, 16, 128], BF16) for i in range(4)]
    xsa = [P.sbuf("xsa%d" % i, [128, D], F32) for i in range(4)]
    xna = [P.sbuf("xna%d" % i, [128, D], BF16) for i in range(4)]
    s3a = [P.sbuf("s3a%d" % i, [128, 4], F32) for i in range(4)]
    ngc3 = P.sbuf("ngc3", [128, 16], F32)
    gate1_bc = xsa[3]
    zt = P.sbuf("zt", [128, D], BF16)
    gb3 = ot[0]
    c3 = {"xs": 0}
    IOA = bass.IndirectOffsetOnAxis

    def bc_rows(colap, dst):
        op("dve", lambda e: e.tensor_copy(out=gb3[:].rearrange("p (j m) -> p j m", m=128), in_=bcl(colap, [128, 8, 128])),
           reads=[modc, G12], writes=[gb3])
        b_, pg3, pg3k = pbank(2)

        def mmg3(e):
            for j in range(8):
                ins = e.matmul(pg3[:, j * 128:(j + 1) * 128], lhsT=gb3[:, j * 128:(j + 1) * 128], rhs=ident_f, start=True, stop=True)
            return ins
        op("pe", mmg3, reads=[gb3, cf], writes=pg3k)
        op("act", lambda e: e.activation(out=dst[:], in_=pg3, func=AF.Copy), reads=pg3k, writes=[dst])
    bc_rows(modc[:, 40:48], gate2_bc)
    bc_rows(G12[:, 8:16], G2_bc)
    bc_rows(modc[:, 24:32], sh2_bc)
    bc_rows(modc[:, 16:24], gate1_bc)
    dma("sp", lambda e: e.dma_start(out=ngc3[:], in_=ngcol[:, :]), "c9", writes=[ngc3])
    for kc in range(16):
        wt = xstg[kc % 2]
        dma("sp", lambda e, wt=wt, kc=kc: e.dma_start(out=wt[:], in_=w_out[kc * 128:(kc + 1) * 128, :]), wt, writes=[wt])
        op("dve", lambda e, wt=wt, kc=kc: e.scalar_tensor_tensor(out=wout3[:, kc, :], in0=wt[:], scalar=ngc3[:, kc:kc + 1],
                                                                in1=gate1_bc[:], op0=ALU.mult, op1=ALU.mult),
           reads=[wt, ngc3, gate1_bc], writes=[(wout3, kc)])
    dma("sp", lambda e: e.dma_start(out=fing_bc[:], in_=fing.partition_broadcast(128)), "c3", writes=[fing_bc])
    dma("sp", lambda e: e.dma_start(out=c2t[:], in_=c2_d[:, :]), "c2", writes=[c2t])
    dma("pool", lambda e: e.dma_start(out=wr_bf[:], in_=w_r.rearrange("(k p) c -> p k c", p=128)), "wr", writes=[wr_bf])
    dma("sp", lambda e: e.dma_start(out=br_bc[:], in_=b_r.partition_broadcast(128)), "br", writes=[br_bc])
    op("dve", lambda e: e.memset(zt[:], 0.0), writes=[zt])
    hsz = hs_d.rearrange("(b p) d -> b p d", p=128)
    for b in range(NSLOT // 128):
        dma("act", lambda e, b=b: e.dma_start(out=hsz[b], in_=zt[:]), "hsz", reads=[zt])
    op("dve", lambda e: e.memset(runc[:], 0.0), writes=[runc])

    def xs_rot():
        i = c3["xs"]
        c3["xs"] += 1
        return xstg[i % 2]

    def router_chunk(c, sid):
        rt = rts[sid]
        h2Tc = h2Tcs[sid]
        prk = [("ps", 2 * sid + 1)]
        pr_ = PS[:, (2 * sid + 1) * 512:(2 * sid + 2) * 512]

        def mm(e):
            for k in range(8):
                ins = e.matmul(pr_[:, 0:36], lhsT=h2Tc[:, k, :], rhs=wr_bf[:, k, :], start=(k == 0), stop=(k == 7))
            return ins
        op("pe", mm, reads=[h2Tc, wr_bf], writes=prk)
        lg = rt[:, 0:36]
        R = [rt]
        op("dve", lambda e: e.tensor_tensor(out=lg, in0=pr_[:, 0:36], in1=br_bc[:], op=ALU.add), reads=prk + [br_bc], writes=R)
        gmax, gsum, pg = rt[:, 36:37], rt[:, 37:38], rt[:, 38:39]
        ohg = rt[:, 40:44]
        op("dve", lambda e: e.reduce_max(out=gmax, in_=rt[:, 0:4], axis=AX.X), reads=R, writes=R)
        op("dve", lambda e: e.tensor_scalar(out=ohg, in0=rt[:, 0:4], scalar1=gmax, scalar2=None, op0=ALU.is_equal), reads=R, writes=R)
        op("dve", lambda e: e.tensor_scalar(out=rt[:, 39:40], in0=gmax, scalar1=-1.0, scalar2=None, op0=ALU.mult), reads=R, writes=R)
        op("act", lambda e: e.activation(out=rt[:, 44:48], in_=rt[:, 0:4], func=AF.Exp, bias=rt[:, 39:40], scale=1.0, accum_out=gsum),
           reads=R, writes=R)
        op("dve", lambda e: e.reciprocal(out=pg, in_=gsum), reads=R, writes=R)
        msk = rt[:, 48:80]
        op("dve", lambda e: e.tensor_tensor(out=msk.rearrange("p (g e) -> p g e", e=8), in0=rt[:, 4:36].rearrange("p (g e) -> p g e", e=8),
                                            in1=bcl(ohg, [128, 4, 8]), op=ALU.mult), reads=R, writes=R)
        esel = rt[:, 80:88]
        op("dve", lambda e: e.tensor_reduce(out=esel, in_=msk.rearrange("p (g e) -> p e g", e=8), axis=AX.X, op=ALU.add), reads=R, writes=R)
        m8 = rt[:, 88:96]
        op("dve", lambda e: e.max(out=m8, in_=esel), reads=R, writes=R)
        op("dve", lambda e: e.tensor_scalar(out=rt[:, 96:97], in0=m8[:, 0:1], scalar1=-1.0, scalar2=None, op0=ALU.mult), reads=R, writes=R)
        tq = rt[:, 100:108]
        op("act", lambda e: e.activation(out=tq, in_=esel, func=AF.Exp, bias=rt[:, 96:97], scale=1.0), reads=R, writes=R)
        mk2 = rt[:, 108:116]
        op("dve", lambda e: e.tensor_scalar(out=mk2, in0=esel, scalar1=m8[:, 1:2], scalar2=None, op0=ALU.is_ge), reads=R, writes=R)
        op("dve", lambda e: e.tensor_tensor(out=tq, in0=tq, in1=mk2, op=ALU.mult), reads=R, writes=R)
        op("dve", lambda e: e.reduce_sum(out=rt[:, 97:98], in_=tq, axis=AX.X), reads=R, writes=R)
        op("dve", lambda e: e.reciprocal(out=rt[:, 98:99], in_=rt[:, 97:98]), reads=R, writes=R)
        op("dve", lambda e: e.tensor_tensor(out=rt[:, 98:99], in0=rt[:, 98:99], in1=pg, op=ALU.mult), reads=R, writes=R)
        op("dve", lambda e: e.tensor_scalar(out=tq, in0=tq, scalar1=rt[:, 98:99], scalar2=None, op0=ALU.mult), reads=R, writes=R)
        op("dve", lambda e: e.tensor_tensor(out=comb_all[:, c, :].rearrange("p (g e) -> p g e", e=8), in0=bcl(ohg, [128, 4, 8]),
                                            in1=bcm(tq, [128, 4, 8]), op=ALU.mult), reads=R, writes=[(comb_all, c)])

    def passA_chunk(c, sid):
        r0 = c * 128
        xs = xsa[sid]
        h2Tc = h2Tcs[sid]
        yt_ = ytc[sid]
        bO, bR = 2 * sid, 2 * sid + 1
        dma("sp", lambda e: e.dma_start(out=xs[:], in_=x[r0:r0 + 128, :]), xs, writes=[xs])
        dma("sp", lambda e: e.dma_start(out=yt_[:], in_=yT_d[c, :, :, :]), yt_, writes=[yt_])
        pok = [("ps", bO)]
        po_ = PS[:, bO * 512:(bO + 1) * 512]
        for n in range(2):
            def mmo(e, n=n):
                for kc in range(16):
                    ins = e.matmul(po_, lhsT=yt_[:, kc, :], rhs=wout3[:, kc, n * 512:(n + 1) * 512], start=(kc == 0), stop=(kc == 15))
                return ins
            op("pe", mmo, reads=[yt_] + [(wout3, kc) for kc in range(16)], writes=pok)
            op("dve", lambda e, n=n: e.tensor_tensor(out=xs[:, n * 512:(n + 1) * 512], in0=po_, in1=xs[:, n * 512:(n + 1) * 512], op=ALU.add),
               reads=pok + [xs], writes=[xs])
        dma("act", lambda e: e.dma_start(out=x1_d[r0:r0 + 128, :], in_=xs[:]), ("x1st", sid), reads=[xs])
        s4 = s3a[sid]
        op("act", lambda e: e.activation(out=junk[:], in_=xs[:], func=AF.Square, accum_out=s4[:, 0:1]), reads=[xs], writes=[s4, "junk"])
        rstd_ops(s4[:, 0:1], s4[:, 1:2], D, [s4])
        xn = xna[sid]
        op("dve", lambda e: e.scalar_tensor_tensor(out=xn[:], in0=xs[:], scalar=s4[:, 1:2], in1=G2_bc[:],
                                                   op0=ALU.mult, op1=ALU.mult), reads=[xs, s4, G2_bc], writes=[xn])
        op("dve", lambda e: e.tensor_tensor(out=h2tok[:, c, :], in0=xn[:], in1=sh2_bc[:], op=ALU.add),
           reads=[xn, sh2_bc], writes=[(h2tok, c)])
        ptk = [("ps", bR)]
        ptb = PS[:, bR * 512:(bR + 1) * 512].bitcast(BF16)

        def tr(e):
            for k in range(8):
                ins = e.transpose(out=ptb[:, k * 128:(k + 1) * 128], in_=h2tok[:, c, k * 128:(k + 1) * 128], identity=ident_bf)
            return ins
        op("pe", tr, reads=[(h2tok, c), cbf], writes=ptk)
        op("act", lambda e: e.activation(out=h2Tc[:].rearrange("p k t -> p (k t)"), in_=ptb, func=AF.Copy), reads=ptk, writes=[h2Tc])
        router_chunk(c, sid)
        oht = ohts[sid]
        op("dve", lambda e: e.tensor_scalar(out=oht[:], in0=comb_all[:, c, :], scalar1=0.0, scalar2=None, op0=ALU.is_gt),
           reads=[(comb_all, c)], writes=[oht])
        pk4 = [("ps", bR)]
        p4 = PS[:, bR * 512:(bR + 1) * 512]
        op("pe", lambda e: e.matmul(p4[:, 64:96], lhsT=SL_f, rhs=oht[:], start=True, stop=True), reads=[oht, cf], writes=pk4)
        op("pe", lambda e: e.matmul(p4[:, 96:128], lhsT=ones_f, rhs=oht[:], start=True, stop=True), reads=[oht, cf] + pk4, writes=pk4)
        op("dve", lambda e: e.tensor_copy(out=pos_all[:, c, :], in_=p4[:, 64:96]), reads=pk4, writes=[(pos_all, c)])
        op("dve", lambda e: e.tensor_copy(out=cnt_all[:, c, :], in_=p4[:, 96:128]), reads=pk4, writes=[(cnt_all, c)])
    for c in range(0, NCH, 4):
        P.interleave([lambda c=c, i=i: passA_chunk(c + i, i) for i in range(4)], [3, 3, 3, 3])
    for c in range(NCH):
        op("dve", lambda e, c=c: e.tensor_tensor(out=pos_all[:, c, :], in0=pos_all[:, c, :], in1=runc[:], op=ALU.add),
           reads=[(pos_all, c), runc], writes=[(pos_all, c)])
        op("dve", lambda e, c=c: e.tensor_tensor(out=runc[:], in0=cnt_all[:, c, :], in1=runc[:], op=ALU.add),
           reads=[(cnt_all, c), runc], writes=[runc])
    op("dve", lambda e: e.tensor_tensor(out=wk3[:], in0=bcl(runc[:], [128, 32, 32]), in1=bcm(thr_bc, [128, 32, 32]), op=ALU.is_gt),
       reads=[runc, c2t], writes=[wk3])
    nt = sm3[:, 32:64]
    op("dve", lambda e: e.tensor_reduce(out=nt, in_=wk3[:], axis=AX.X, op=ALU.add), reads=[wk3], writes=[sm3])
    Dm = sm3[0:32, 64:96]
    op("dve", lambda e: e.tensor_tensor(out=Dm, in0=ident_f[0:32, 0:32], in1=sm3[0:32, 32:64], op=ALU.mult), reads=[sm3, cf], writes=[sm3])
    colv = sm3[0:32, 96:97]
    op("dve", lambda e: e.reduce_sum(out=colv, in_=Dm, axis=AX.X), reads=[sm3], writes=[sm3])
    lbt = wk3[0:32, 0, :].rearrange("p (a b) -> p a b", b=32)
    lb = wk3[0:32, 0:4, :].rearrange("p a b -> p (a b)")
    op("dve", lambda e: e.tensor_copy(out=lb, in_=colv.to_broadcast([32, 128])), reads=[sm3, wk3], writes=[wk3])
    pk4 = [("ps", 4)]
    p4 = PS[:, 4 * 512:5 * 512]
    op("pe", lambda e: e.matmul(p4[:, 64:96], lhsT=lb, rhs=SL_f[0:32, 0:32], start=True, stop=True), reads=[wk3, cf], writes=pk4)
    op("dve", lambda e: e.tensor_scalar(out=segs[:], in0=p4[:, 64:96], scalar1=float(TSL), scalar2=None, op0=ALU.mult), reads=pk4, writes=[segs])
    cumi = sm3[:, 128:160]
    op("dve", lambda e: e.tensor_tensor(out=cumi, in0=p4[:, 64:96], in1=nt, op=ALU.add), reads=pk4 + [sm3], writes=[sm3])
    cmpj = sm3[:, 160:192]
    op("dve", lambda e: e.tensor_scalar(out=cmpj, in0=cumi, scalar1=pcol, scalar2=None, op0=ALU.is_le), reads=[sm3, c2t], writes=[sm3])
    ej = sm3[:, 192:193]
    op("dve", lambda e: e.reduce_sum(out=ej, in_=cmpj, axis=AX.X), reads=[sm3], writes=[sm3])
    op("dve", lambda e: e.tensor_scalar(out=ej, in0=ej, scalar1=31.0, scalar2=None, op0=ALU.min), reads=[sm3], writes=[sm3])
    dgj = wk3[:, 1, :]
    dgj = wk3[:, 4:8, :].rearrange("p a b -> p (a b)")
    op("dve", lambda e: e.tensor_scalar(out=dgj, in0=ident_f, scalar1=ej, scalar2=None, op0=ALU.mult), reads=[sm3, cf, wk3], writes=[wk3])
    op("pe", lambda e: e.matmul(p4[:, 128:256], lhsT=ones_f, rhs=dgj, start=True, stop=True), reads=[wk3, cf] + pk4, writes=pk4)
    idxf = wk3[:, 8:12, :].rearrange("p a b -> p (a b)")
    op("dve", lambda e: e.tensor_scalar(out=idxf, in0=p4[:, 128:256], scalar1=128.0, scalar2=pcol, op0=ALU.mult, op1=ALU.add),
       reads=pk4 + [c2t, wk3], writes=[wk3])
    op("dve", lambda e: e.tensor_copy(out=IDXW[:], in_=idxf), reads=[wk3], writes=[IDXW])
    op("dve", lambda e: e.tensor_scalar(out=idxf, in0=idxf, scalar1=2.0, scalar2=None, op0=ALU.mult), reads=[wk3, IDXW], writes=[wk3])
    op("dve", lambda e: e.tensor_copy(out=IDXA[:], in_=idxf), reads=[wk3], writes=[IDXA])
    op("dve", lambda e: e.tensor_scalar(out=idxf, in0=idxf, scalar1=1.0, scalar2=None, op0=ALU.add), reads=[wk3, IDXA], writes=[wk3])
    op("dve", lambda e: e.tensor_copy(out=IDXB[:], in_=idxf), reads=[wk3], writes=[IDXB])
    P.flush()
    def passB_chunk(c, sid):
        rt = rts[sid]
        eqt = ohts[sid]
        R = [rt]
        sf, oht, ms, m8 = rt[:, 0:32], rt[:, 32:64], rt[:, 64:96], rt[:, 96:104]
        op("dve", lambda e: e.tensor_tensor(out=sf, in0=pos_all[:, c, :], in1=segs[:], op=ALU.add), reads=[(pos_all, c), segs] + R, writes=R)
        op("dve", lambda e: e.tensor_scalar(out=oht, in0=comb_all[:, c, :], scalar1=0.0, scalar2=None, op0=ALU.is_gt),
           reads=[(comb_all, c)] + R, writes=R)
        op("dve", lambda e: e.scalar_tensor_tensor(out=ms, in0=sf, scalar=1.0, in1=oht, op0=ALU.add, op1=ALU.mult), reads=R, writes=R)
        op("dve", lambda e: e.max(out=m8, in_=ms), reads=R, writes=R)
        for k in range(2):
            eq = eqt[:]
            op("dve", lambda e, k=k: e.tensor_scalar(out=eq, in0=ms, scalar1=m8[:, k:k + 1], scalar2=None, op0=ALU.is_equal),
               reads=R + [eqt], writes=[eqt])
            op("dve", lambda e: e.tensor_tensor(out=eq, in0=eq, in1=comb_all[:, c, :], op=ALU.mult), reads=[eqt, (comb_all, c)], writes=[eqt])
            op("dve", lambda e, k=k: e.reduce_sum(out=w_all[:, c, k:k + 1], in_=eq, axis=AX.X), reads=[eqt], writes=[(w_all, c)])
        op("dve", lambda e: e.tensor_scalar(out=rt[:, 104:106], in0=m8[:, 0:2], scalar1=-1.0, scalar2=None, op0=ALU.add), reads=R, writes=R)
        op("dve", lambda e: e.tensor_copy(out=idx_all[:, c, :], in_=rt[:, 104:106]), reads=R, writes=[(idx_all, c)])
        for k in range(2):
            if stop == "idx":
                continue
            dma("pool", lambda e, k=k: e.indirect_dma_start(out=hs_d[:, :], out_offset=IOA(ap=idx_all[:, c, k:k + 1], axis=0),
                                                            in_=h2tok[:, c, :], in_offset=None),
                "hsc", reads=[(idx_all, c), (h2tok, c)])
    for c in range(0, NCH, 2):
        P.interleave([lambda c=c: passB_chunk(c, 0), lambda c=c: passB_chunk(c + 1, 1)], [3, 3])
    if stop == "idx":
        dump("idx_all", idx_all[:], [(idx_all, c) for c in range(NCH)])
        dump("w_all", w_all[:], [(w_all, c) for c in range(NCH)])
        dump("IDXW", IDXW[:], [IDXW])
        dump("segs", segs[:], [segs])
        dump("runc", runc[:], [runc])
        dump("comb_all", comb_all[:], [(comb_all, c) for c in range(NCH)])
        dump("pos_all", pos_all[:], [(pos_all, c) for c in range(NCH)])
        P.flush()
        ph3a.close()
        ph3.close()
        P.base.close()
        return nc
    P.flush()
    if stop == "scatter":
        ph3a.close()
        ph3.close()
        P.base.close()
        return nc
    ph3a.close()
    ph3t = contextlib.ExitStack()
    P.scope = ph3t
    w13t = [P.sbuf("w13t%d" % i, [128, 8, 512], BF16) for i in range(3)]
    w2t = [P.sbuf("w2t%d" % i, [128, 2, D], BF16) for i in range(3)]
    hst = [P.sbuf("hst%d" % i, [128, D], BF16) for i in range(4)]
    hsT = [P.sbuf("hsT%d" % i, [128, 8, 128], BF16) for i in range(4)]
    s1t = [P.sbuf("s1t%d" % i, [128, 256], BF16) for i in range(4)]
    at = [P.sbuf("at%d" % i, [128, 256], BF16) for i in range(4)]
    aTt = [P.sbuf("aTt%d" % i, [128, 256], BF16) for i in range(4)]
    yst = [P.sbuf("yst%d" % i, [128, D], BF16) for i in range(2)]
    w13v = w13c_d
    w2v = w2c_d

    def load_w(j):
        b = j % 3
        for hf, IX in ((0, IDXA), (1, IDXB)):
            dma("pool", lambda e, hf=hf, IX=IX: e.indirect_dma_start(
                out=w13t[b][:, 4 * hf:4 * hf + 4, :].rearrange("p k c -> p (k c)"), out_offset=None, in_=w13v[:, :],
                in_offset=IOA(ap=IX[:, j:j + 1], axis=0)), ("w13", b), reads=[IX], writes=[w13t[b]])
        dma("pool", lambda e: e.indirect_dma_start(out=w2t[b][:].rearrange("p h c -> p (h c)"), out_offset=None, in_=w2v[:, :],
                                                   in_offset=IOA(ap=IDXW[:, j:j + 1], axis=0)), ("w2", b), reads=[IDXW], writes=[w2t[b]])
    NI = NTL * (TSL // 128)
    SPT = TSL // 128

    def st_L(i):
        r0 = i * 128
        hs_ = hst[i % 4]
        dma("sp", lambda e: e.dma_start(out=hs_[:], in_=hs_d[r0:r0 + 128, :]), hs_, writes=[hs_])

    def st_T(i):
        q4 = i % 4
        hs_ = hst[q4]
        bT = 6 + i % 2
        ptk = [("ps", bT)]
        ptb = PS[:, bT * 512:(bT + 1) * 512].bitcast(BF16)

        def tr(e):
            for k in range(8):
                ins = e.transpose(out=ptb[:, k * 128:(k + 1) * 128], in_=hs_[:, k * 128:(k + 1) * 128], identity=ident_bf)
            return ins
        op("pe", tr, reads=[hs_, cbf], writes=ptk)
        hT_ = hsT[q4]
        op("act", lambda e: e.activation(out=hT_[:].rearrange("p k t -> p (k t)"), in_=ptb, func=AF.Copy), reads=ptk, writes=[hT_])

    def st_H(i):
        q4 = i % 4
        b = (i // SPT) % 3
        hT_ = hsT[q4]
        bA = 4 + i % 2
        ph_, phk = PS[:, bA * 512:(bA + 1) * 512], [("ps", bA)]

        def mmh(e):
            for k in range(8):
                ins = e.matmul(ph_, lhsT=hT_[:, k, :], rhs=w13t[b][:, k, :], start=(k == 0), stop=(k == 7))
            return ins
        op("pe", mmh, reads=[hT_, w13t[b]], writes=phk)
        s1, a_ = s1t[q4], at[q4]
        op("act", lambda e: e.activation(out=s1[:], in_=ph_[:, 0:256], func=AF.Silu), reads=phk, writes=[s1])
        op("dve", lambda e: e.tensor_tensor(out=a_[:], in0=ph_[:, 256:512], in1=s1[:], op=ALU.mult), reads=phk + [s1], writes=[a_])

    def st_B(i):
        q4 = i % 4
        a_, aT_ = at[q4], aTt[q4]
        bA = 4 + i % 2
        pt7k = [("ps", bA)]
        pt7b = PS[:, bA * 512:bA * 512 + 128].bitcast(BF16)

        def tr2(e):
            for hh in range(2):
                ins = e.transpose(out=pt7b[:, hh * 128:(hh + 1) * 128], in_=a_[:, hh * 128:(hh + 1) * 128], identity=ident_bf)
            return ins
        op("pe", tr2, reads=[a_, cbf], writes=pt7k)
        op("act", lambda e: e.activation(out=aT_[:], in_=pt7b[:, 0:256], func=AF.Copy), reads=pt7k, writes=[aT_])

    def st_Y(i):
        q4 = i % 4
        q = i % 2
        b = (i // SPT) % 3
        r0 = i * 128
        aT_ = aTt[q4]
        pa, pak = PS[:, q * 1024:(q + 1) * 1024], [("ps", 2 * q), ("ps", 2 * q + 1)]

        def mmy(e):
            for n in range(2):
                for hh in range(2):
                    ins = e.matmul(pa[:, n * 512:(n + 1) * 512], lhsT=aT_[:, hh * 128:(hh + 1) * 128],
                                   rhs=w2t[b][:, hh, n * 512:(n + 1) * 512], start=(hh == 0), stop=(hh == 1))
            return ins
        op("pe", mmy, reads=[aT_, w2t[b]], writes=pak)
        ys_ = yst[q]
        op("act", lambda e: e.activation(out=ys_[:], in_=pa, func=AF.Copy), reads=pak, writes=[ys_])
        dma("pool", lambda e: e.dma_start(out=ys_d[r0:r0 + 128, :], in_=ys_[:]), ("yst", q), reads=[ys_])

    load_w(0)
    load_w(1)
    st_L(0)
    st_L(1)
    for n in range(NI + 3):
        if n + 2 < NI:
            st_L(n + 2)
        if n < NI:
            st_T(n)
        if 0 <= n - 1 < NI:
            st_H(n - 1)
        if 0 <= n - 2 < NI:
            st_B(n - 2)
        if 0 <= n - 3 < NI:
            st_Y(n - 3)
        if n % SPT == 0 and n >= SPT:
            jn = n // SPT + 1
            if jn < NTL:
                load_w(jn)
    P.flush()
    if stop == "tiles":
        ph3t.close()
        ph3.close()
        P.base.close()
        return nc
    ph3t.close()
    ph3c = contextlib.ExitStack()
    P.scope = ph3c
    yg = [P.sbuf("yg%d" % i, [128, D], BF16) for i in range(8)]
    otc = [P.sbuf("otc%d" % i, [128, D], F32) for i in range(4)]
    xsc = [P.sbuf("xsc%d" % i, [128, D], F32) for i in range(4)]
    s3c = [P.sbuf("s3c%d" % i, [128, 4], F32) for i in range(4)]
    def combine_chunk(c, sid):
        r0 = c * 128
        o_ = otc[sid]
        s4 = s3c[sid]
        xs = xsc[sid]
        dma("sp", lambda e: e.dma_start(out=xs[:], in_=x1_d[r0:r0 + 128, :]), xs, writes=[xs])
        ygs = [yg[2 * sid + k] for k in range(2)]
        for k in range(2):
            dma("pool", lambda e, k=k: e.indirect_dma_start(out=ygs[k][:], out_offset=None, in_=ys_d[:, :],
                                                            in_offset=IOA(ap=idx_all[:, c, k:k + 1], axis=0)),
                ygs[k], reads=[(idx_all, c)], writes=[ygs[k]])
        op("act", lambda e: e.activation(out=o_[:], in_=ygs[0][:], func=AF.Copy, scale=w_all[:, c, 0:1]),
           reads=[ygs[0], (w_all, c)], writes=[o_])
        op("dve", lambda e: e.scalar_tensor_tensor(out=o_[:], in0=ygs[1][:], scalar=w_all[:, c, 1:2], in1=o_[:],
                                                   op0=ALU.mult, op1=ALU.add), reads=[ygs[1], (w_all, c), o_], writes=[o_])
        op("dve", lambda e: e.tensor_tensor(out=o_[:], in0=o_[:], in1=gate2_bc[:], op=ALU.mult), reads=[o_, gate2_bc], writes=[o_])
        op("dve", lambda e: e.tensor_tensor(out=o_[:], in0=o_[:], in1=xs[:], op=ALU.add), reads=[o_, xs], writes=[o_])
        op("act", lambda e: e.activation(out=junk[:], in_=o_[:], func=AF.Square, accum_out=s4[:, 2:3]), reads=[o_, s4], writes=[s4, "junk"])
        rstd_ops(s4[:, 2:3], s4[:, 3:4], D, [s4])
        op("dve", lambda e: e.scalar_tensor_tensor(out=o_[:], in0=o_[:], scalar=s4[:, 3:4], in1=fing_bc[:],
                                                   op0=ALU.mult, op1=ALU.mult), reads=[o_, s4, fing_bc], writes=[o_])
        dma("act", lambda e: e.dma_start(out=out[r0:r0 + 128, :], in_=o_[:]), ("ost", sid), reads=[o_])
    for c in range(0, NCH, 4):
        P.interleave([lambda c=c, i=i: combine_chunk(c + i, i) for i in range(4)], [3, 3, 3, 3])
    P.flush()
    ph3c.close()
    ph3.close()
    P.base.close()
    return nc


def _consts():
    i = np.arange(128)
    ident = np.eye(128, dtype=np.float32)
    ones = np.ones((128, 128), np.float32)
    U = (i[:, None] <= i[None, :]).astype(np.float32)
    L = (i[:, None] >= i[None, :]).astype(np.float32)
    SL = (i[:, None] < i[None, :]).astype(np.float32)
    mF = np.where(i[:, None] <= i[None, :], 0.0, NEG).astype(np.float32)
    mB = np.where(i[:, None] >= i[None, :], 0.0, NEG).astype(np.float32)
    cbf = np.stack([ident, ones, U, L, mF, mB], axis=1).astype(ml_dtypes.bfloat16)
    cf = np.stack([ident, ones, U, L, SL], axis=1).astype(np.float32)
    return np.ascontiguousarray(cbf), np.ascontiguousarray(cf)


def _cols(v, n):
    return np.ascontiguousarray(np.asarray(v, np.float32).reshape(n, 128).T)


def prep_inputs(inp, b):
    f = lambda a: np.ascontiguousarray(np.asarray(a, np.float32))
    cbf, cf = _consts()
    m = {}
    m["x"] = f(inp["x"][b])
    m["ccol"] = _cols(inp["c"][b], 8)
    m["w_ada"] = f(inp["w_ada"][0])
    m["badac"] = _cols(inp["b_ada"][0], 48)
    m["n12g"] = np.ascontiguousarray(np.concatenate([_cols(inp["norm1_g"][0], 8), _cols(inp["norm2_g"][0], 8)], axis=1))
    m["w_in"] = f(inp["w_in"][0])
    m["b_in"] = f(inp["b_in"][0]).reshape(1, -1)
    m["bxbc"] = _cols(np.asarray(inp["b_in"][0])[3072:4608], 12)
    m["lngb"] = np.ascontiguousarray(np.stack([f(inp["gmlp_ln_g"][0]), f(inp["gmlp_ln_b"][0])], axis=0))
    m["w_sT"] = np.ascontiguousarray(np.transpose(f(inp["gmlp_w_s"][0]), (2, 0, 1)))
    m["b_s2"] = np.ascontiguousarray(f(inp["gmlp_b_s"][0]).T)
    m["ngcol"] = np.ascontiguousarray(np.concatenate([_cols(inp["gmlp_out_g"][0], 8), _cols(inp["ssd_norm_g"][0], 8)], axis=1))
    cw = f(inp["conv_w"][0])
    m["convw"] = np.ascontiguousarray(np.transpose(cw.reshape(5, 12, 128), (2, 1, 0)))
    m["convb"] = _cols(inp["conv_b"][0], 12)
    m["ssdv"] = np.ascontiguousarray(np.concatenate([f(inp["a_log_f"][0]), f(inp["a_log_b"][0]), f(inp["dt_bias_f"][0]),
                                                     f(inp["dt_bias_b"][0])]).reshape(1, 64))
    m["dskc"] = _cols(np.repeat(f(inp["d_skip"][0]), 64), 8)
    m["w_out"] = f(inp["w_out"][0])
    wre = f(inp["w_router_e"][0])
    m["w_r"] = np.ascontiguousarray(np.concatenate([f(inp["w_router_g"][0])] + [wre[g] for g in range(4)], axis=1))
    m["b_r"] = np.ascontiguousarray(np.concatenate([f(inp["b_router_g"][0]), f(inp["b_router_e"][0]).reshape(-1)]).reshape(1, 36))
    w13 = np.concatenate([f(inp["w1"][0]), f(inp["w3"][0])], axis=2)
    m["w13p"] = np.ascontiguousarray(np.transpose(w13.reshape(32, 8, 128, 512), (0, 2, 1, 3)).reshape(8192, 2048))
    m["w2p"] = np.ascontiguousarray(np.transpose(f(inp["w2"][0]).reshape(32, 2, 128, D), (0, 2, 1, 3)).reshape(4096, 2048))
    c2 = np.zeros((128, 34), np.float32)
    c2[:, 0:32] = 256.0 * np.arange(32, dtype=np.float32)[None, :]
    c2[:, 32] = np.arange(128, dtype=np.float32)
    m["c2"] = c2
    m["fing"] = f(inp["final_g"]).reshape(1, -1)
    m["cbf"] = cbf
    m["cf"] = cf
    return m


_NC_CACHE = {}


def kernel(**inputs):
    x = np.asarray(inputs["x"])
    B, S, _ = x.shape
    if S not in _NC_CACHE:
        _NC_CACHE[S] = build_nc(S)
    nc = _NC_CACHE[S]
    shared = None
    in_maps = []
    for b in range(B):
        if shared is None:
            m = prep_inputs(inputs, b)
            shared = m
        else:
            m = dict(shared)
            m["x"] = np.ascontiguousarray(x[b], dtype=np.float32)
            m["ccol"] = _cols(np.asarray(inputs["c"])[b], 8)
        in_maps.append(m)
    res = run_bass_kernel_spmd(nc, in_maps, core_ids=list(range(B)))
    return np.stack([np.asarray(r["out"], dtype=np.float32) for r in res.results], axis=0)
```
